# Optimizing a Trainium2 kernel written in Bass

```python
import jax, jax.numpy as jnp
from jax import lax
import numpy as np

D_MODEL = 2048
BATCH = 4
SEQ = 8192
DEPTH = 2

EPS = 1e-6
N_BRANCH = 3
BRANCH_WIDTH = D_MODEL // 4
SGU_GROUPS = 8
SGU_GROUP_DIM = BRANCH_WIDTH // SGU_GROUPS
SGU_CHUNK = 128
MOBA_HEADS = 8
MOBA_HEAD_DIM = BRANCH_WIDTH // MOBA_HEADS
MOBA_BLOCK = 256
MOBA_TOPK = 3
MOBA_QBLOCK = 64
POOL_WINDOWS = (2, 4, 8, 16)
POOL_GROUPS = 4
POOL_GROUP_DIM = BRANCH_WIDTH // POOL_GROUPS
IN_SPLIT_POINTS = tuple(BRANCH_WIDTH * i for i in range(1, 7))
IN_WIDTH = 6 * BRANCH_WIDTH + N_BRANCH * D_MODEL
D_FF = 11 * D_MODEL // 4
CONV_WIDTH = 3
N_MOD = 6

kernel_name = "hybrid_sgu_moba_pool_block"


def rms_norm(x, g):
    xf = x.astype(jnp.float32)
    y = xf * lax.rsqrt(jnp.mean(xf * xf, axis=-1, keepdims=True) + EPS)
    return (y * g.astype(jnp.float32)).astype(x.dtype)


def alibi_slopes(n_heads):
    return jnp.exp2(-(8.0 / n_heads) * jnp.arange(1, n_heads + 1, dtype=jnp.float32))


def spatial_gating(u, v, norm_g, w_s, b_s):
    Bsz, S, _ = u.shape
    vf = v.astype(jnp.float32)
    mu = jnp.mean(vf, axis=-1, keepdims=True)
    var = jnp.mean(jnp.square(vf - mu), axis=-1, keepdims=True)
    vn = (vf - mu) * lax.rsqrt(var + EPS) * norm_g.astype(jnp.float32)
    vc = vn.reshape(Bsz, S // SGU_CHUNK, SGU_CHUNK, SGU_GROUPS, SGU_GROUP_DIM)
    mask = jnp.tril(jnp.ones((SGU_CHUNK, SGU_CHUNK), jnp.float32))
    w = w_s.astype(jnp.float32) * mask
    mixed = jnp.einsum('gts,bcsgd->bctgd', w, vc) + b_s.astype(jnp.float32).T[None, None, :, :, None]
    return u * mixed.reshape(Bsz, S, BRANCH_WIDTH).astype(u.dtype)


def moba_attention(q, k, v):
    Bsz, S, _ = q.shape
    H, Dh, BLK, QB = MOBA_HEADS, MOBA_HEAD_DIM, MOBA_BLOCK, MOBA_QBLOCK
    nb = -(-S // BLK)
    pad = nb * BLK - S

    def heads(t):
        return t.reshape(Bsz, S, H, Dh).transpose(0, 2, 1, 3)

    qh = heads(q)
    kb = jnp.pad(heads(k), ((0, 0), (0, 0), (0, pad), (0, 0))).reshape(Bsz, H, nb, BLK, Dh)
    vb = jnp.pad(heads(v), ((0, 0), (0, 0), (0, pad), (0, 0))).reshape(Bsz, H, nb, BLK, Dh)
    kmean = jnp.mean(kb.astype(jnp.float32), axis=3)
    slopes = alibi_slopes(H)[None, :, None, None]
    k_sel = min(MOBA_TOPK, nb)
    scale = Dh ** -0.5
    bi = jnp.arange(Bsz)[:, None, None, None]
    hi = jnp.arange(H)[None, :, None, None]
    key_off = jnp.arange(BLK)
    n_q = S // QB
    q_chunks = qh.reshape(Bsz, H, n_q, QB, Dh).transpose(2, 0, 1, 3, 4)

    def attend(args):
        qc, ci = args
        t = ci * QB + jnp.arange(QB)
        own = (ci * QB) // BLK
        blk_scores = jnp.einsum('bhqd,bhnd->bhqn', qc.astype(jnp.float32), kmean)
        blk_scores = jnp.where(jnp.arange(nb) < own, blk_scores, -jnp.inf)
        _, idx = lax.top_k(blk_scores, k_sel)
        ksel = kb[bi, hi, idx]
        vsel = vb[bi, hi, idx]
        s_past = jnp.einsum('bhqd,bhqjsd->bhqjs', qc, ksel,
                            preferred_element_type=jnp.float32) * scale
        pos_past = idx[..., None] * BLK + key_off
        dist_past = (t[None, None, :, None, None] - pos_past).astype(jnp.float32)
        s_past = s_past - slopes[..., None] * dist_past
        valid = (jnp.arange(k_sel) < own)[:, None]
        s_past = jnp.where(valid, s_past, -jnp.inf).reshape(Bsz, H, QB, k_sel * BLK)
        k_own = lax.dynamic_index_in_dim(kb, own, axis=2, keepdims=False)
        v_own = lax.dynamic_index_in_dim(vb, own, axis=2, keepdims=False)
        dist_own = t[:, None] - (own * BLK + key_off)[None, :]
        s_own = jnp.einsum('bhqd,bhsd->bhqs', qc, k_own,
                           preferred_element_type=jnp.float32) * scale
        s_own = s_own - slopes * dist_own.astype(jnp.float32)
        s_own = jnp.where(dist_own >= 0, s_own, -jnp.inf)
        p = jax.nn.softmax(jnp.concatenate([s_past, s_own], axis=-1), axis=-1)
        p_past = p[..., :k_sel * BLK].reshape(Bsz, H, QB, k_sel, BLK).astype(v.dtype)
        p_own = p[..., k_sel * BLK:].astype(v.dtype)
        o = (jnp.einsum('bhqjs,bhqjsd->bhqd', p_past, vsel, preferred_element_type=jnp.float32)
             + jnp.einsum('bhqs,bhsd->bhqd', p_own, v_own, preferred_element_type=jnp.float32))
        return o.astype(v.dtype)

    out = lax.map(attend, (q_chunks, jnp.arange(n_q)))
    return out.transpose(1, 0, 3, 2, 4).reshape(Bsz, S, H * Dh)


def multiscale_pool(z, pool_w, pool_scale):
    Bsz, S, _ = z.shape
    zf = z.astype(jnp.float32).reshape(Bsz, S, POOL_GROUPS, POOL_GROUP_DIM)
    cs = jnp.pad(jnp.cumsum(zf, axis=1), ((0, 0), (1, 0), (0, 0), (0, 0)))
    t = jnp.arange(S)
    means = []
    for gi, w in enumerate(POOL_WINDOWS):
        lo = jnp.maximum(t + 1 - w, 0)
        win_sum = cs[:, 1:, gi] - jnp.take(cs[:, :, gi], lo, axis=1)
        count = jnp.minimum(t + 1, w).astype(jnp.float32)
        means.append(win_sum / count[None, :, None])
    pooled = jnp.stack(means, axis=2) - zf
    y = jnp.einsum('bsgc,gcd->bsgd', pooled, pool_w.astype(jnp.float32))
    return (y.reshape(Bsz, S, BRANCH_WIDTH) * pool_scale.astype(jnp.float32)).astype(z.dtype)


def token_mixing(h, w_in, b_in, sgu_norm_g, sgu_w, sgu_b, pool_w, pool_scale,
                 w_sgu_out, w_moba_out, w_pool_out, w_out):
    p = h @ w_in + b_in
    u, v, q, k, v_att, z_pool, gates = jnp.split(p, IN_SPLIT_POINTS, axis=-1)
    y_sgu = spatial_gating(jax.nn.gelu(u), jax.nn.gelu(v), sgu_norm_g, sgu_w, sgu_b)
    y_moba = moba_attention(q, k, v_att)
    y_pool = multiscale_pool(z_pool, pool_w, pool_scale)
    g_sgu, g_moba, g_pool = jnp.split(jax.nn.sigmoid(gates), N_BRANCH, axis=-1)
    merged = (g_sgu * (y_sgu @ w_sgu_out) + g_moba * (y_moba @ w_moba_out)
              + g_pool * (y_pool @ w_pool_out))
    return merged @ w_out


def conv_ffn(h, w_up, w_conv, b_conv, w_down):
    z = h @ w_up
    z1 = jnp.pad(z[:, :-1], ((0, 0), (1, 0), (0, 0)))
    z2 = jnp.pad(z[:, :-2], ((0, 0), (2, 0), (0, 0)))
    z = w_conv[0] * z2 + w_conv[1] * z1 + w_conv[2] * z + b_conv
    gate, val = jnp.split(z, 2, axis=-1)
    return (jax.nn.gelu(gate) * val) @ w_down


def setup_inputs(seed: int = 0) -> dict:
    key = jax.random.key(seed)
    ks = jax.random.split(key, 24)
    f32 = jnp.float32
    L, D = DEPTH, D_MODEL

    def nrm(k, shape, s):
        return jax.random.normal(k, shape, f32) * s

    return {
        "x": nrm(ks[0], (BATCH, SEQ, D), 1.0),
        "c": nrm(ks[1], (BATCH, D), 1.0),
        "g_pre_mix": 1.0 + nrm(ks[2], (L, D), 0.02),
        "g_post_mix": 1.0 + nrm(ks[3], (L, D), 0.02),
        "g_pre_ffn": 1.0 + nrm(ks[4], (L, D), 0.02),
        "g_post_ffn": 1.0 + nrm(ks[5], (L, D), 0.02),
        "w_ada": nrm(ks[6], (L, D, N_MOD * D), D ** -0.5),
        "b_ada": nrm(ks[7], (L, N_MOD * D), 0.01),
        "w_in": nrm(ks[8], (L, D, IN_WIDTH), D ** -0.5),
        "b_in": nrm(ks[9], (L, IN_WIDTH), 0.01),
        "sgu_norm_g": 1.0 + nrm(ks[10], (L, BRANCH_WIDTH), 0.02),
        "sgu_w": nrm(ks[11], (L, SGU_GROUPS, SGU_CHUNK, SGU_CHUNK), SGU_CHUNK ** -0.5),
        "sgu_b": 1.0 + nrm(ks[12], (L, SGU_GROUPS, SGU_CHUNK), 0.02),
        "pool_w": nrm(ks[13], (L, POOL_GROUPS, POOL_GROUP_DIM, POOL_GROUP_DIM), POOL_GROUP_DIM ** -0.5),
        "pool_scale": 1.0 + nrm(ks[14], (L, BRANCH_WIDTH), 0.02),
        "w_sgu_out": nrm(ks[15], (L, BRANCH_WIDTH, D), BRANCH_WIDTH ** -0.5),
        "w_moba_out": nrm(ks[16], (L, BRANCH_WIDTH, D), BRANCH_WIDTH ** -0.5),
        "w_pool_out": nrm(ks[17], (L, BRANCH_WIDTH, D), BRANCH_WIDTH ** -0.5),
        "w_out": nrm(ks[18], (L, D, D), D ** -0.5),
        "w_up": nrm(ks[19], (L, D, 2 * D_FF), D ** -0.5),
        "w_conv": nrm(ks[20], (L, CONV_WIDTH, 2 * D_FF), CONV_WIDTH ** -0.5),
        "b_conv": nrm(ks[21], (L, 2 * D_FF), 0.01),
        "w_down": nrm(ks[22], (L, D_FF, D), D_FF ** -0.5),
    }


def reference(x, c, g_pre_mix, g_post_mix, g_pre_ffn, g_post_ffn, w_ada, b_ada,
              w_in, b_in, sgu_norm_g, sgu_w, sgu_b, pool_w, pool_scale,
              w_sgu_out, w_moba_out, w_pool_out, w_out, w_up, w_conv, b_conv, w_down):
    cond = jax.nn.silu(c)
    for l in range(DEPTH):
        mod = cond @ w_ada[l] + b_ada[l]
        sh1, sc1, gt1, sh2, sc2, gt2 = [m[:, None, :] for m in jnp.split(mod, N_MOD, axis=-1)]
        h = rms_norm(x, g_pre_mix[l]) * (1.0 + sc1) + sh1
        y = token_mixing(h, w_in[l], b_in[l], sgu_norm_g[l], sgu_w[l], sgu_b[l],
                         pool_w[l], pool_scale[l], w_sgu_out[l], w_moba_out[l],
                         w_pool_out[l], w_out[l])
        x = x + gt1 * rms_norm(y, g_post_mix[l])
        h = rms_norm(x, g_pre_ffn[l]) * (1.0 + sc2) + sh2
        y = conv_ffn(h, w_up[l], w_conv[l], b_conv[l], w_down[l])
        x = x + gt2 * rms_norm(y, g_post_ffn[l])
    return x
```

```python
import os
import numpy as np
import ml_dtypes
from contextlib import ExitStack
import concourse.bass as bass
import concourse.mybir as mybir
from concourse.bass_utils import run_bass_kernel_spmd

F32, BF16 = mybir.dt.float32, mybir.dt.bfloat16
AF = mybir.ActivationFunctionType
ALU = mybir.AluOpType
AX = mybir.AxisListType

D = 2048
KC = 16
TT = 512
DFF = 5632
FC = 44
NBP = 32
EPS = 1e-6
BIGZ = 240000.0
GELU = AF.Gelu_apprx_tanh


class T:
    def __init__(s, name, dg=None):
        s.name = name; s.w = {}; s.r = {}; s.dg = dg


class DS:
    def __init__(s, h):
        s.h = h; s.val = 0


class KB:
    def __init__(s, nc, es):
        s.nc = nc; s.es = es
        s.E = {'pe': nc.tensor, 'act': nc.scalar, 'dve': nc.vector, 'pool': nc.gpsimd, 'sp': nc.sync}
        s.sem = {e: es.enter_context(nc.semaphore('s_' + e)) for e in s.E}
        s.cnt = {e: 0 for e in s.E}
        s.seen = {e: {} for e in s.E}
        s.dsems = {}
        s.psi = 0

    def _wait(s, e, evs):
        for key, val in evs.items():
            if key == ('e', e) and e == 'pe':
                continue
            if key[0] == 'd':
                val = s.dsems[key[1]].val
            if s.seen[e].get(key, 0) >= val:
                continue
            h = s.sem[key[1]] if key[0] == 'e' else s.dsems[key[1]].h
            s.E[e].wait_ge(h, val)
            s.seen[e][key] = val

    def _deps(s, e, r, w):
        evs = {}
        def add(d):
            for k, v in d.items():
                if evs.get(k, 0) < v:
                    evs[k] = v
        for t in r:
            add(t.w)
            if getattr(t, 'psum', False):
                add(t.r)
        for t in w:
            add(t.w); add(t.r)
        s._wait(e, evs)

    def _rec(s, key, val, r, w):
        for t in r:
            t.r[key] = max(t.r.get(key, 0), val)
        for t in w:
            t.w = {key: val}; t.r = {}

    def op(s, e, fn, r, w):
        s._deps(e, r, w)
        inst = fn(s.E[e])
        s.cnt[e] += 1
        inst.then_inc(s.sem[e], 1)
        s._rec(('e', e), s.cnt[e], r, w)

    def mm(s, ps, out_ap, pairs, r, start=True, stop=True):
        s._deps('pe', r, [ps])
        n = len(pairs)
        inst = None
        for i, (a, b) in enumerate(pairs):
            inst = s.nc.tensor.matmul(out_ap, a, b, start=(start and i == 0), stop=(stop and i == n - 1))
        s.cnt['pe'] += 1
        inst.then_inc(s.sem['pe'], 1)
        s._rec(('e', 'pe'), s.cnt['pe'], r, [ps])

    def tr(s, ps, out_ap, in_ap, ident_ap, r):
        s._deps('pe', r, [ps])
        inst = s.nc.tensor.transpose(out_ap, in_ap, ident_ap)
        s.cnt['pe'] += 1
        inst.then_inc(s.sem['pe'], 1)
        s._rec(('e', 'pe'), s.cnt['pe'], r, [ps])

    def dma(s, q, out_ap, in_ap, r, w, **kw):
        s._deps(q, r, w)
        g = (w[0].dg or w[0].name) + '_' + q
        fifo = s.__dict__.setdefault('fifo_' + q, [])
        if len(fifo) >= 6:
            s._wait(q, {('d', fifo.pop(0)): 0})
        fifo.append(g)
        if g not in s.dsems:
            s.dsems[g] = DS(s.es.enter_context(s.nc.semaphore('d_' + g)))
        ds = s.dsems[g]
        s.E[q].dma_start(out=out_ap, in_=in_ap, **kw).then_inc(ds.h, 16)
        ds.val += 16
        s._rec(('d', g), ds.val, r, w)

    def barrier(s):
        evs = {('e', e): s.cnt[e] for e in s.E if s.cnt[e] > 0}
        evs.update({('d', g): ds.val for g, ds in s.dsems.items() if ds.val > 0})
        for e in s.E:
            s._wait(e, evs)


def build(SEQ, DEPTH):
    NT = SEQ // TT
    NKT = SEQ // 128
    nc = bass.Bass("TRN2", target_bir_lowering=False)
    es = ExitStack()
    k = KB(nc, es)

    def din(name, shape, dt=F32):
        return nc.dram_tensor(name, list(shape), dt, kind="ExternalInput").ap()

    def dscr(name, shape, dt):
        return nc.dram_tensor(name, list(shape), dt).ap()

    def sb(name, shape, dt=F32):
        return es.enter_context(nc.sbuf_tensor("sb_" + name, list(shape), dt))

    xT_in = din("xT", [KC, 128, SEQ])
    outT = nc.dram_tensor("outT", [KC, 128, SEQ], F32, kind="ExternalOutput").ap()
    cT_d = din("cT", [128, KC])
    gn_d = din("gn", [DEPTH, 128, 4 * KC])
    bada_d = din("bada", [DEPTH, 128, 96])
    bin_d = din("binT", [DEPTH, 128, 72])
    bvB_d = din("bvB", [DEPTH, 128, 512])
    bvaB_d = din("bvaB", [DEPTH, 128, 512])
    sngB_d = din("sngB", [DEPTH, 128, 512])
    sbT_d = din("sbT", [DEPTH, 128, 4, 128])
    swT_d = din("swT", [DEPTH, 128, 8, 128])
    pw_d = din("pw", [DEPTH, 128, 4, 128])
    psc_d = din("psc", [DEPTH, 128, 4])
    cw_d = din("cw", [DEPTH, 128, 88 * 3])
    cb_d = din("cb", [DEPTH, 128, 88])
    WA_d = din("WA", [DEPTH, 48, 128, 2 * KC * 128])
    WI_d = din("WI", [DEPTH, 6, 128, 4 * KC * 128])
    WM_d = din("WM", [DEPTH, 16, 128, 7680])
    WO_d = din("WO", [DEPTH, 4, 128, 4 * KC * 128])
    WU_d = din("WU", [DEPTH, 22, 128, 4 * KC * 128])
    WD_d = din("WD", [DEPTH, 16, 128, FC * 128])
    kaux_d = din("kaux", [8, 36, SEQ], BF16)
    qaux_d = din("qaux", [8, 4, SEQ], BF16)
    blkb_d = din("blkb", [128, NBP, NBP])
    owni_d = din("owni", [128, NBP, NBP])
    cm_d = din("cm", [128, 4, 512], BF16)
    ones_d = din("onesb", [128, 128], BF16)
    ident_d = din("ident", [128, 128], BF16)
    shsel_d = din("shsel", [128, 64], BF16)
    invc_d = din("invc", [128, 4, 512])
    triu_d = din("triu", [128, 128])

    Xs = dscr("Xs", [KC, 128, SEQ], F32)
    QT16 = dscr("QT16", [4, 128, SEQ], BF16)
    KT16 = dscr("KT16", [4, 128, SEQ], BF16)
    V16 = dscr("V16", [NKT, 128, 512], BF16)
    MB = dscr("MB", [8, NBP, SEQ], BF16)
    YM = dscr("YM", [4, 128, SEQ], BF16)

    XinT = [T("xin%d" % i, dg="xin") for i in range(NT)]
    XsT = [T("xs%d" % i, dg="xsd") for i in range(NT)]
    XoT = [T("xo%d" % i, dg="xod") for i in range(NT)]
    QT16t, KT16t, V16t, MBt, YMt = T("QT16"), T("KT16"), T("V16"), T("MB"), T("YM")
    CONST = T("const")

    ps_t = [es.enter_context(nc.psum_tensor("ps%d" % i, [128, 512], F32)) for i in range(8)]
    ps_T = [T("ps%d" % i) for i in range(8)]
    for t_ in ps_T:
        t_.psum = True

    def PS():
        i = k.psi; k.psi = (k.psi + 1) % 6
        return ps_T[i], ps_t[i]
    ACC_T, ACC = ps_T[6], ps_t[6]
    AUX_T, AUX = ps_T[7], ps_t[7]

    onesb = sb("onesb", [128, 128], BF16)
    ident = sb("ident", [128, 128], BF16)
    shsel = sb("shsel", [128, 64], BF16)
    triu = sb("triu", [128, 128])
    cm = sb("cm", [128, 4, 512], BF16)
    blkb = sb("blkb", [128, 2, NBP]); owni = sb("owni", [128, 2, NBP]); BO_T = T("blkown")
    cT = sb("cT", [128, KC])
    condT = sb("condT", [128, KC], BF16)
    cst = [(onesb, ones_d), (ident, ident_d), (shsel, shsel_d), (triu, triu_d), (cm, cm_d),
           (cT, cT_d)]
    for t_, d_ in cst:
        k.dma('sp', t_[:], d_, [], [CONST])
    k.op('act', lambda e: e.activation(out=condT[:], in_=cT[:], func=AF.Silu), [CONST], [CONST])

    gn = sb("gn", [128, 4 * KC]); bada = sb("bada", [128, 96]); binT = sb("binT", [128, 72])
    bvB = sb("bvB", [128, 512]); bvaB = sb("bvaB", [128, 512]); sngB = sb("sngB", [128, 512])
    sbT = sb("sbT", [128, 4, 128]); swm = sb("swm", [128, 8, 128], BF16)
    pwb = sb("pwb", [128, 4, 128], BF16); psc = sb("psc", [128, 4])
    cw = sb("cw", [128, 88 * 3]); cb = sb("cb", [128, 88])
    modT = sb("modT", [128, 96])
    G1 = sb("G1", [128, KC]); GG1 = sb("GG1", [128, KC]); G2 = sb("G2", [128, KC]); GG2 = sb("GG2", [128, KC])
    kmT = sb("kmT", [128, 4, NBP])
    LP = T("lparams")

    NSLOT = 3
    slots = [sb("slot%d" % i, [128, 8192], BF16) for i in range(NSLOT)]
    slotT = [T("slot%d" % i) for i in range(NSLOT)]
    sl = {'i': 0}

    def wload(src_ap, nelem, cast=True):
        i = sl['i']; sl['i'] = (i + 1) % NSLOT
        if cast and not os.environ.get('KNOW'):
            k.dma('pool', slots[i][:, 0:nelem], src_ap, [], [slotT[i]], max_dma_last_dim=4096)
        return slots[i], slotT[i]

    Wb = dscr("Wb", [DEPTH, 64, 128, 8192], BF16)
    WbT = [[T("wb%d_%d" % (l_, g_), dg="wb") for g_ in range(64)] for l_ in range(DEPTH)]
    WOFF = {'I': 0, 'M': 6, 'O': 22, 'U': 26, 'D': 48}
    WSRC = {'I': (WI_d, 6, 8192), 'M': (WM_d, 16, 7680), 'O': (WO_d, 4, 8192), 'U': (WU_d, 22, 8192), 'D': (WD_d, 16, FC * 128)}

    def wl(kind, l_, idx):
        n = WSRC[kind][2]
        g = WOFF[kind] + idx
        i = sl['i']; sl['i'] = (i + 1) % NSLOT
        k.dma('pool', slots[i][:, 0:n], Wb[l_, g, :, 0:n], [WbT[l_][g]], [slotT[i]])
        return slots[i], slotT[i]

    for l_ in range(DEPTH):
        for kind in ('I', 'M', 'O', 'U', 'D'):
            src, cnt, n = WSRC[kind]
            for idx in range(cnt):
                g = WOFF[kind] + idx
                WbT[l_][g].dg = "wbs%d" % sl['i']
                slot, sT = wload(src[l_, idx], n)
                k.dma('sp', Wb[l_, g, :, 0:n], slot[:, 0:n], [sT], [WbT[l_][g]])

    xTf = sb("xTt", [128, KC * TT]); xT_T = T("xTt")
    xT = xTf[:].rearrange("p (c t) -> p c t", c=KC)
    XB = xTf[:].bitcast(BF16)
    hT = sb("hTt", [128, KC, TT], BF16); hT_T = T("hTt")
    yT = sb("yTt", [128, KC, TT], BF16); yT_T = T("yTt")
    aT = sb("aTt", [128, FC, TT], BF16); aT_T = T("aTt")
    tmpAx = sb("tmpA", [128, TT + 16]); tmpA_T = T("tmpA")
    tmpBx = sb("tmpB", [128, TT + 16]); tmpB_T = T("tmpB")
    tmpA = tmpAx[:, 0:TT]; tmpB = tmpBx[:, 0:TT]
    tmpC = sb("tmpC", [128, TT]); tmpC_T = T("tmpC")
    tmpD = sb("tmpD", [128, TT]); tmpD_T = T("tmpD")
    sq = [sb("sq%d" % i, [128, TT], BF16) for i in range(2)]; sq_T = [T("sq%d" % i) for i in range(2)]
    rstdB = sb("rstdB", [128, TT]); rstdB_T = T("rstdB")
    st6 = sb("st6", [128, 6]); mv2 = sb("mv2", [128, 2]); sm_T = T("small")
    zb = [sb("zb%d" % i, [128, TT + 2]) for i in range(2)]; zb_T = [T("zb%d" % i) for i in range(2)]
    carry = sb("carry", [128, 88, 2]); carry_T = T("carry")
    zp = [sb("zp%d" % i, [128, TT + 16]) for i in range(2)]; zp_T = [T("zp%d" % i) for i in range(2)]
    halo = sb("halo", [128, 4, 16]); halo_T = T("halo")
    pa, pa_T, pb, pb_T = tmpAx, tmpA_T, tmpBx, tmpB_T
    pbufT = [T("pbuf%d" % i) for i in range(4)]
    KM_T = T("kmT")
    kmH = sb("kmH", [128, 4, NBP], BF16); kmL = sb("kmL", [128, 4, NBP], BF16)
    qlo = sb("qlo", [128, TT], BF16); qlo_T = T("qlo")
    mbb = sb("mbb", [128, 4 * NBP], BF16); mbb_T = T("mbb")
    hl_T = T("hl")

    guT = aT[:, 0:4, :]; ysT = aT[:, 4:8, :]; ymT = aT[:, 8:12, :]; ypT = aT[:, 12:16, :]
    mgT = aT[:, 16:32, :]; vnT = aT[:, 32:36, :]; pT4 = aT[:, 36:40, :]; plT = aT[:, 40:44, :]
    gu_T, ys_T, ym_T, yp_T, mg_T, vn_T, p4_T, pl_T = (T("gu"), T("ys"), T("ym"), T("yp"), T("mg"), T("vn"), T("p4"), T("pl"))
    KTa = XB[:, 0:SEQ]; KTa_T = T("KTa")
    Va = XB[:, 8192:8192 + NKT * 128].rearrange("p (k c) -> p k c", c=128); Va_T = T("Va")
    QTa = [sb("QTa%d" % i, [128, TT], BF16) for i in range(2)]; QTa_T = [T("QTa%d" % i) for i in range(2)]

    def stats_rstd(src_tile, src_T, nch):
        for c in range(nch):
            j = c % 2
            k.op('act', lambda e, c=c, j=j: e.activation(out=sq[j][:], in_=src_tile[:, c, :], func=AF.Square), [src_T], [sq_T[j]])
            k.mm(ACC_T, ACC[:], [(onesb[:], sq[j][:])], [sq_T[j], CONST], start=(c == 0), stop=(c == nch - 1))
        k.op('dve', lambda e: e.tensor_scalar(out=tmpD[:], in0=ACC[:], scalar1=1.0 / D, scalar2=EPS, op0=ALU.mult, op1=ALU.add), [ACC_T], [tmpD_T])
        k.op('act', lambda e: e.activation(out=tmpD[:], in_=tmpD[:], func=AF.Sqrt), [tmpD_T], [tmpD_T])
        k.op('dve', lambda e: e.reciprocal(out=rstdB[:], in_=tmpD[:]), [tmpD_T], [rstdB_T])

    def make_h(Gs, SHcol):
        stats_rstd(xT, xT_T, KC)
        for c in range(KC):
            tt, tT = (tmpA, tmpA_T) if c % 2 == 0 else (tmpB, tmpB_T)
            k.op('dve', lambda e, c=c, tt=tt: e.scalar_tensor_tensor(out=tt[:], in0=xT[:, c, :], scalar=Gs[:, c:c + 1], in1=rstdB[:], op0=ALU.mult, op1=ALU.mult), [xT_T, rstdB_T, LP], [tT])
            k.op('act', lambda e, c=c, tt=tt: e.activation(out=hT[:, c, :], in_=tt[:], func=AF.Identity, bias=modT[:, SHcol + c:SHcol + c + 1], scale=1.0), [tT, LP], [hT_T])

    def proj_fm(slot, sT, j, rhs_tile, rhs_T, nk, kstride):
        pT_, p_ = PS()
        w = slot[:, j * nk * 128:(j + 1) * nk * 128]
        k.mm(pT_, p_[:], [(w[:, kc * 128:(kc + 1) * 128], rhs_tile[:, kc, :]) for kc in range(nk)], [sT, rhs_T])
        return pT_, p_

    def post_norm_residual(GGs):
        k.op('dve', lambda e: e.tensor_scalar(out=tmpD[:], in0=ACC[:], scalar1=1.0 / D, scalar2=EPS, op0=ALU.mult, op1=ALU.add), [ACC_T], [tmpD_T])
        k.op('act', lambda e: e.activation(out=tmpD[:], in_=tmpD[:], func=AF.Sqrt), [tmpD_T], [tmpD_T])
        k.op('dve', lambda e: e.reciprocal(out=rstdB[:], in_=tmpD[:]), [tmpD_T], [rstdB_T])
        for c in range(KC):
            tt, tT = (tmpA, tmpA_T) if c % 2 == 0 else (tmpB, tmpB_T)
            k.op('dve', lambda e, c=c, tt=tt: e.scalar_tensor_tensor(out=tt[:], in0=yT[:, c, :], scalar=GGs[:, c:c + 1], in1=rstdB[:], op0=ALU.mult, op1=ALU.mult), [yT_T, rstdB_T, LP], [tT])
            k.op('dve', lambda e, c=c, tt=tt: e.tensor_tensor(out=xT[:, c, :], in0=xT[:, c, :], in1=tt[:], op=ALU.add), [tT, xT_T], [xT_T])

    def evac_y(pT_, p_, oc, first, last):
        k.op('act', lambda e: e.activation(out=yT[:, oc, :], in_=p_[:], func=AF.Copy), [pT_], [yT_T])
        j = oc % 2
        k.op('act', lambda e: e.activation(out=sq[j][:], in_=p_[:], func=AF.Square), [pT_], [sq_T[j]])
        k.mm(ACC_T, ACC[:], [(onesb[:], sq[j][:])], [sq_T[j], CONST], start=first, stop=last)

    for l in range(DEPTH):
        Xsrc, XsrcT = (xT_in, XinT) if l == 0 else (Xs, XsT)
        Xdst, XdstT = (outT, XoT) if l == DEPTH - 1 else (Xs, XsT)
        k.barrier()
        for t_, d_ in [(gn, gn_d), (bada, bada_d), (binT, bin_d), (bvB, bvB_d), (bvaB, bvaB_d), (sngB, sngB_d),
                       (sbT, sbT_d), (psc, psc_d), (cw, cw_d), (cb, cb_d)]:
            k.dma('sp', t_[:], d_[l], [], [LP])
        k.dma('pool', pwb[:], pw_d[l], [], [LP])
        for hf in range(2):
            k.dma('sp', tmpA[:].rearrange('p (g t) -> p g t', g=4), swT_d[l][:, 4 * hf:4 * hf + 4, :], [], [tmpA_T])
            for g4 in range(4):
                k.op('dve', lambda e, g4=g4, hf=hf: e.tensor_tensor(out=swm[:, 4 * hf + g4, :], in0=tmpA[:, g4 * 128:(g4 + 1) * 128], in1=triu[:], op=ALU.mult), [tmpA_T, CONST], [LP])
        k.op('dve', lambda e: e.memset(kmT[:], 0.0), [], [KM_T])
        k.op('dve', lambda e: e.memset(kmH[:], 0.0), [KM_T], [KM_T])
        k.op('dve', lambda e: e.memset(kmL[:], 0.0), [KM_T], [KM_T])
        k.op('dve', lambda e: e.memset(carry[:], 0.0), [], [carry_T])
        k.op('dve', lambda e: e.memset(halo[:], 0.0), [], [halo_T])
        for jj in range(48):
            slot, sT = wload(WA_d[l, jj], 4096)
            for h2 in range(2):
                j = 2 * jj + h2
                k.mm(AUX_T, AUX[:, j:j + 1], [(slot[:, (h2 * KC + kc) * 128:(h2 * KC + kc + 1) * 128], condT[:, kc:kc + 1]) for kc in range(KC)], [sT, CONST])
        k.op('dve', lambda e: e.tensor_tensor(out=modT[:], in0=AUX[:, 0:96], in1=bada[:], op=ALU.add), [AUX_T, LP], [LP])
        k.op('dve', lambda e: e.scalar_tensor_tensor(out=G1[:], in0=modT[:, 16:32], scalar=1.0, in1=gn[:, 0:16], op0=ALU.add, op1=ALU.mult), [LP], [LP])
        k.op('dve', lambda e: e.tensor_tensor(out=GG1[:], in0=modT[:, 32:48], in1=gn[:, 16:32], op=ALU.mult), [LP], [LP])
        k.op('dve', lambda e: e.scalar_tensor_tensor(out=G2[:], in0=modT[:, 64:80], scalar=1.0, in1=gn[:, 32:48], op0=ALU.add, op1=ALU.mult), [LP], [LP])
        k.op('dve', lambda e: e.tensor_tensor(out=GG2[:], in0=modT[:, 80:96], in1=gn[:, 48:64], op=ALU.mult), [LP], [LP])

        if os.environ.get('KSTOP') == 'ada':
            k.barrier(); return nc, es
        for ti in range(NT):
            c0 = ti * TT
            for q4 in range(4):
                k.dma('sp', xT[:, 4 * q4:4 * q4 + 4, :], Xsrc[4 * q4:4 * q4 + 4, :, c0:c0 + TT].rearrange("c p t -> p c t"), [XsrcT[ti]], [xT_T])
            make_h(G1, 0)
            k.dma('sp', blkb[:], blkb_d[:, 2 * ti:2 * ti + 2, :], [], [BO_T])
            k.dma('sp', owni[:], owni_d[:, 2 * ti:2 * ti + 2, :], [], [BO_T])
            SECT = os.environ.get('KSECT', 'K,Q,QS,V,KD,QD,VD,MBD,TR').split(',')
            slot, sT = wl('I', l, 2)
            for c in range(4 if 'K' in SECT else 0):
                pT_, p_ = proj_fm(slot, sT, c, hT, hT_T, KC, 0)
                bcol = binT[:, 12 + c:13 + c]
                k.op('act', lambda e, p_=p_, c=c, bcol=bcol: e.activation(out=aT[:, c, :], in_=p_[:], func=AF.Identity, bias=bcol, scale=1.0), [pT_, LP], [p4_T])
                k.op('dve', lambda e, p_=p_: e.tensor_reduce(out=mv2[:], in_=p_[:].rearrange("p (b s) -> p b s", s=256), axis=AX.X, op=ALU.add), [pT_], [sm_T])
                k.op('dve', lambda e: e.tensor_scalar(out=mv2[:], in0=mv2[:], scalar1=1.0 / 256, scalar2=None, op0=ALU.mult), [sm_T], [sm_T])
                k.op('dve', lambda e, c=c, bcol=bcol: e.tensor_scalar(out=kmT[:, c, 2 * ti:2 * ti + 2], in0=mv2[:], scalar1=bcol, scalar2=None, op0=ALU.add), [sm_T, LP], [KM_T])
                k.op('dve', lambda e, c=c: e.tensor_copy(out=kmH[:, c, :], in_=kmT[:, c, :]), [KM_T], [KM_T])
                k.op('dve', lambda e, c=c: e.tensor_tensor(out=kmL[:, c, :], in0=kmT[:, c, :], in1=kmH[:, c, :], op=ALU.subtract), [KM_T], [KM_T])
            if 'KD' in SECT:
                for c in range(4):
                    k.dma('sp', KT16[c, :, c0:c0 + TT], aT[:, c, :], [p4_T], [KT16t])
            slot, sT = wl('I', l, 1)
            for c in range(4 if 'Q' in SECT else 0):
                pT_, p_ = proj_fm(slot, sT, c, hT, hT_T, KC, 0)
                bcol = binT[:, 8 + c:9 + c]
                k.op('act', lambda e, p_=p_, c=c, bcol=bcol: e.activation(out=aT[:, 4 + c, :], in_=p_[:], func=AF.Identity, bias=bcol, scale=1.0), [pT_, LP], [pl_T])
                k.op('dve', lambda e, p_=p_, bcol=bcol: e.tensor_scalar(out=tmpC[:], in0=p_[:], scalar1=bcol, scalar2=None, op0=ALU.add), [pT_, LP], [tmpC_T])
                k.op('dve', lambda e, c=c: e.tensor_tensor(out=qlo[:], in0=tmpC[:], in1=aT[:, 4 + c, :], op=ALU.subtract), [tmpC_T, pl_T], [qlo_T])
                for hh in range(2 if 'QS' in SECT else 0):
                    h = 2 * c + hh
                    r0 = 64 * hh
                    sT_, s_ = PS()
                    for su in range(4):
                        qh = aT[r0:r0 + 64, 4 + c, su * 128:(su + 1) * 128]; ql = qlo[r0:r0 + 64, su * 128:(su + 1) * 128]
                        k.mm(sT_, s_[:, su * NBP:(su + 1) * NBP], [(qh, kmH[r0:r0 + 64, c, :]), (ql, kmH[r0:r0 + 64, c, :]), (qh, kmL[r0:r0 + 64, c, :])], [qlo_T, pl_T, KM_T])
                    for su in range(4):
                        k.op('dve', lambda e, s_=s_, su=su: e.tensor_tensor(out=tmpA[:, su * NBP:(su + 1) * NBP], in0=s_[:, su * NBP:(su + 1) * NBP], in1=blkb[:, su // 2, :], op=ALU.add), [sT_, BO_T], [tmpA_T])
                    for su in range(4):
                        seg = tmpA[:, su * NBP:(su + 1) * NBP]
                        k.op('dve', lambda e, seg=seg: e.max(out=tmpB[:, 0:8], in_=seg), [tmpA_T], [tmpB_T])
                        k.op('dve', lambda e: e.tensor_scalar(out=tmpB[:, 8:9], in0=tmpB[:, 2:3], scalar1=-1e29, scalar2=None, op0=ALU.max), [tmpB_T], [tmpB_T])
                        k.op('dve', lambda e, seg=seg: e.tensor_scalar(out=seg, in0=seg, scalar1=tmpB[:, 8:9], scalar2=None, op0=ALU.is_ge), [tmpB_T, tmpA_T], [tmpA_T])
                        k.op('dve', lambda e, seg=seg, su=su: e.tensor_tensor(out=seg, in0=seg, in1=owni[:, su // 2, :], op=ALU.max), [tmpA_T, BO_T], [tmpA_T])
                        k.op('dve', lambda e, seg=seg, su=su: e.tensor_scalar(out=mbb[:, su * NBP:(su + 1) * NBP], in0=seg, scalar1=-1.0, scalar2=BIGZ, op0=ALU.add, op1=ALU.mult), [tmpA_T], [mbb_T])
                    if 'TR' not in SECT:
                        continue
                    tT_, t_ = PS()
                    for su in range(4):
                        k.tr(tT_, t_[:].bitcast(BF16)[0:NBP, su * 128:(su + 1) * 128], mbb[:, su * NBP:(su + 1) * NBP], ident[:], [mbb_T, CONST])
                    j = h % 2
                    k.op('act', lambda e, t_=t_, j=j: e.activation(out=sq[j][0:NBP, :], in_=t_[:].bitcast(BF16)[0:NBP, 0:TT], func=AF.Copy), [tT_], [sq_T[j]])
                    if 'MBD' in SECT:
                        k.dma('sp', MB[h, :, c0:c0 + TT], sq[j][0:NBP, :], [sq_T[j]], [MBt])
            if 'QD' in SECT:
                for c in range(4):
                    k.dma('sp', QT16[c, :, c0:c0 + TT], aT[:, 4 + c, :], [pl_T], [QT16t])
            slot, sT = wl('I', l, 3)
            wv = slot[:].rearrange("p (j k c) -> p j k c", j=4, k=KC)
            for su in range(4 if 'V' in SECT else 0):
                pT_, p_ = PS()
                k.mm(pT_, p_[:].rearrange("p (j c) -> p j c", j=4), [(hT[:, kc, su * 128:(su + 1) * 128], wv[:, :, kc, :]) for kc in range(KC)], [sT, hT_T])
                k.op('dve', lambda e, p_=p_, su=su: e.tensor_tensor(out=aT[:, 8 + su, :], in0=p_[:], in1=bvaB[:], op=ALU.add), [pT_, LP], [ym_T])
                kt = ti * 4 + su
                if 'VD' not in SECT:
                    continue
                k.dma('sp', V16[kt], aT[:, 8 + su, :], [ym_T], [V16t])

        if os.environ.get('KSTOP') == 'A':
            k.barrier(); return nc, es
        k.barrier()
        k.op('dve', lambda e: e.memset(Va[:, :, 64:128], 1.0), [], [Va_T])
        for h in range(8):
            c, r0 = h // 2, 64 * (h % 2)
            k.dma('sp', KTa[0:64, :], KT16[c, r0:r0 + 64, :], [KT16t], [KTa_T])
            k.dma('sp', KTa[64:100, :], kaux_d[h], [], [KTa_T])
            for k8 in range(0, NKT, 8):
                k.dma('sp', Va[:, k8:k8 + 8, 0:64], V16[k8:k8 + 8, :, h * 64:(h + 1) * 64].rearrange("k p d -> p k d"), [V16t], [Va_T])
            for ti in range(NT):
                c0 = ti * TT
                qi = (h * NT + ti) % 2
                Q, Q_T = QTa[qi], QTa_T[qi]
                k.dma('sp', Q[0:64, :], QT16[c, r0:r0 + 64, c0:c0 + TT], [QT16t], [Q_T])
                k.dma('sp', Q[64:96, :], MB[h, :, c0:c0 + TT], [MBt], [Q_T])
                k.dma('sp', Q[96:100, :], qaux_d[h, :, c0:c0 + TT], [], [Q_T])
                nkt = 4 * ti + 4
                for kt in range(nkt):
                    sT_, s_ = PS()
                    k.mm(sT_, s_[:], [(KTa[0:100, kt * 128:(kt + 1) * 128], Q[0:100, :])], [KTa_T, Q_T])
                    pj = kt % 4
                    P = hT[:, pj, :]
                    PTt = pbufT[pj]
                    if kt >= 4 * ti:
                        k.op('dve', lambda e, s_=s_: e.tensor_scalar(out=tmpC[:], in0=s_[:], scalar1=400.0, scalar2=None, op0=ALU.min), [sT_], [tmpC_T])
                        k.op('act', lambda e, P=P: e.activation(out=P, in_=tmpC[:], func=AF.Exp, scale=0.125), [tmpC_T], [PTt])
                    else:
                        k.op('act', lambda e, s_=s_, P=P: e.activation(out=P, in_=s_[:], func=AF.Exp, scale=0.125), [sT_], [PTt])
                    if kt >= 4 * ti:
                        k.op('dve', lambda e, P=P, kt=kt: e.tensor_tensor(out=P, in0=P, in1=cm[:, kt - 4 * ti, :], op=ALU.mult), [PTt, CONST], [PTt])
                    k.mm(ACC_T, ACC[:], [(Va[:, kt, :], P)], [Va_T, PTt], start=(kt == 0), stop=(kt == nkt - 1))
                k.op('act', lambda e: e.activation(out=tmpA[:], in_=ACC[:], func=AF.Copy), [ACC_T], [tmpA_T])
                k.op('dve', lambda e: e.tensor_copy(out=hT[:, 4, :], in_=tmpA[:]), [tmpA_T], [hl_T])
                k.op('dve', lambda e: e.tensor_tensor(out=hT[:, 5, :], in0=tmpA[:], in1=hT[:, 4, :], op=ALU.subtract), [tmpA_T, hl_T], [hl_T])
                k.mm(AUX_T, AUX[0:64, :], [(shsel[:], hT[:, 4, :]), (shsel[:], hT[:, 5, :])], [hl_T, CONST])
                k.op('dve', lambda e: e.reciprocal(out=tmpB[0:64, :], in_=AUX[0:64, :]), [AUX_T], [tmpB_T])
                j = ti % 2
                k.op('dve', lambda e, j=j: e.tensor_tensor(out=sq[j][0:64, :], in0=tmpA[0:64, :], in1=tmpB[0:64, :], op=ALU.mult), [tmpA_T, tmpB_T], [sq_T[j]])
                k.dma('sp', YM[c, r0:r0 + 64, c0:c0 + TT], sq[j][0:64, :], [sq_T[j]], [YMt])

        if os.environ.get('KSTOP') == 'B':
            k.barrier(); return nc, es
        k.barrier()
        for ti in range(NT):
            c0 = ti * TT
            for q4 in range(4):
                k.dma('sp', xT[:, 4 * q4:4 * q4 + 4, :], Xsrc[4 * q4:4 * q4 + 4, :, c0:c0 + TT].rearrange("c p t -> p c t"), [XsrcT[ti]], [xT_T])
            k.dma('sp', ymT, YM[:, :, c0:c0 + TT].rearrange("c p t -> p c t"), [YMt], [ym_T])
            make_h(G1, 0)
            slot, sT = wl('I', l, 0)
            for c in range(4):
                pT_, p_ = proj_fm(slot, sT, c, hT, hT_T, KC, 0)
                k.op('act', lambda e, p_=p_, c=c: e.activation(out=aT[:, c, :], in_=p_[:], func=GELU, bias=binT[:, c:c + 1], scale=1.0), [pT_, LP], [gu_T])
            slot, sT = wl('I', l, 4)
            wv = slot[:].rearrange("p (j k c) -> p j k c", j=4, k=KC)
            for su in range(4):
                pT_, p_ = PS()
                k.mm(pT_, p_[:].rearrange("p (j c) -> p j c", j=4), [(hT[:, kc, su * 128:(su + 1) * 128], wv[:, :, kc, :]) for kc in range(KC)], [sT, hT_T])
                k.op('dve', lambda e, p_=p_: e.tensor_tensor(out=tmpA[:], in0=p_[:], in1=bvB[:], op=ALU.add), [pT_, LP], [tmpA_T])
                k.op('act', lambda e: e.activation(out=tmpA[:], in_=tmpA[:], func=GELU), [tmpA_T], [tmpA_T])
                k.op('dve', lambda e: e.bn_stats(out=st6[:], in_=tmpA[:]), [tmpA_T], [sm_T])
                k.op('dve', lambda e: e.bn_aggr(out=mv2[:], in_=st6[:]), [sm_T], [sm_T])
                k.op('dve', lambda e: e.tensor_scalar(out=mv2[:, 1:2], in0=mv2[:, 1:2], scalar1=EPS, scalar2=None, op0=ALU.add), [sm_T], [sm_T])
                k.op('act', lambda e: e.activation(out=mv2[:, 1:2], in_=mv2[:, 1:2], func=AF.Sqrt), [sm_T], [sm_T])
                k.op('dve', lambda e: e.reciprocal(out=mv2[:, 1:2], in_=mv2[:, 1:2]), [sm_T], [sm_T])
                k.op('dve', lambda e: e.tensor_scalar(out=tmpA[:], in0=tmpA[:], scalar1=mv2[:, 0:1], scalar2=mv2[:, 1:2], op0=ALU.subtract, op1=ALU.mult), [sm_T, tmpA_T], [tmpA_T])
                k.op('dve', lambda e, su=su: e.tensor_tensor(out=aT[:, 32 + su, :], in0=tmpA[:], in1=sngB[:], op=ALU.mult), [tmpA_T, LP], [vn_T])
            for cc in range(4):
                pT_, p_ = PS()
                for su in range(4):
                    for gg in range(2):
                        g = 2 * cc + gg
                        k.mm(pT_, p_[64 * gg:64 * gg + 64, su * 128:(su + 1) * 128], [(aT[:, 32 + su, g * 64:(g + 1) * 64], swm[:, g, :])], [vn_T, LP])
                for su in range(4):
                    k.op('dve', lambda e, p_=p_, cc=cc, su=su: e.tensor_tensor(out=tmpA[:, su * 128:(su + 1) * 128], in0=p_[:, su * 128:(su + 1) * 128], in1=sbT[:, cc, :], op=ALU.add), [pT_, LP], [tmpA_T])
                k.op('dve', lambda e, cc=cc: e.tensor_tensor(out=aT[:, 4 + cc, :], in0=tmpA[:], in1=aT[:, cc, :], op=ALU.mult), [tmpA_T, gu_T], [ys_T])
            slot, sT = wl('I', l, 5)
            for gi in range(4):
                w = (2, 4, 8, 16)[gi]
                pT_, p_ = proj_fm(slot, sT, gi, hT, hT_T, KC, 0)
                Z, Z_T = zp[gi % 2], zp_T[gi % 2]
                k.op('act', lambda e, Z=Z, gi=gi: e.activation(out=Z[:, 0:16], in_=halo[:, gi, :], func=AF.Copy), [halo_T], [Z_T])
                k.op('act', lambda e, p_=p_, Z=Z, gi=gi: e.activation(out=Z[:, 16:16 + TT], in_=p_[:], func=AF.Identity, bias=binT[:, 20 + gi:21 + gi], scale=1.0), [pT_, LP], [Z_T])
                bufs = [(pa, pa_T), (pb, pb_T)]
                src, src_T = Z, Z_T
                step = 1; bi = 0; lo = 0
                while step < w:
                    lo += step
                    dst, dst_T = bufs[bi]; bi ^= 1
                    k.op('dve', lambda e, dst=dst, src=src, lo=lo, step=step: e.tensor_tensor(out=dst[:, lo:TT + 16], in0=src[:, lo:TT + 16], in1=src[:, lo - step:TT + 16 - step], op=ALU.add), [src_T], [dst_T])
                    src, src_T = dst, dst_T
                    step *= 2
                if ti == 0:
                    k.dma('sp', tmpD[:], invc_d[:, gi, :], [], [tmpD_T])
                    k.op('dve', lambda e, src=src, gi=gi: e.tensor_tensor(out=tmpC[:], in0=src[:, 16:16 + TT], in1=tmpD[:], op=ALU.mult), [src_T, tmpD_T], [tmpC_T])
                    k.op('dve', lambda e, Z=Z, gi=gi: e.tensor_tensor(out=aT[:, 40 + gi, :], in0=tmpC[:], in1=Z[:, 16:16 + TT], op=ALU.subtract), [tmpC_T, Z_T], [pl_T])
                else:
                    k.op('dve', lambda e, src=src, Z=Z, gi=gi, w=w: e.scalar_tensor_tensor(out=aT[:, 40 + gi, :], in0=src[:, 16:16 + TT], scalar=1.0 / w, in1=Z[:, 16:16 + TT], op0=ALU.mult, op1=ALU.subtract), [src_T, Z_T], [pl_T])
                k.op('act', lambda e, Z=Z, gi=gi: e.activation(out=halo[:, gi, :], in_=Z[:, TT:TT + 16], func=AF.Copy), [Z_T], [halo_T])
                qT_, q_ = PS()
                k.mm(qT_, q_[:], [(pwb[:, gi, :], aT[:, 40 + gi, :])], [pl_T, LP])
                k.op('act', lambda e, q_=q_, gi=gi: e.activation(out=aT[:, 12 + gi, :], in_=q_[:], func=AF.Identity, scale=psc[:, gi:gi + 1]), [qT_, LP], [yp_T])
            brs = [(4, ys_T), (8, ym_T), (12, yp_T)]
            for oc in range(16):
                slot, sT = wl('M', l, oc)
                for br in range(3):
                    gT_, g_ = PS()
                    wg = slot[:, br * 2048:(br + 1) * 2048]
                    k.mm(gT_, g_[:], [(wg[:, kc * 128:(kc + 1) * 128], hT[:, kc, :]) for kc in range(KC)], [sT, hT_T])
                    gcol = 24 + br * 16 + oc
                    gt_, gtT = [(tmpA, tmpA_T), (tmpB, tmpB_T), (tmpC, tmpC_T)][br]
                    k.op('act', lambda e, g_=g_, gt_=gt_, gcol=gcol: e.activation(out=gt_[:], in_=g_[:], func=AF.Sigmoid, bias=binT[:, gcol:gcol + 1], scale=1.0), [gT_, LP], [gtT])
                    bT_, b_ = PS()
                    wb = slot[:, 6144 + br * 512:6144 + (br + 1) * 512]
                    a0, aT_ = brs[br]
                    k.mm(bT_, b_[:], [(wb[:, kc * 128:(kc + 1) * 128], aT[:, a0 + kc, :]) for kc in range(4)], [sT, aT_])
                    k.op('dve', lambda e, b_=b_, gt_=gt_: e.tensor_tensor(out=gt_[:], in0=b_[:], in1=gt_[:], op=ALU.mult), [bT_, gtT], [gtT])
                k.op('dve', lambda e: e.tensor_tensor(out=tmpA[:], in0=tmpA[:], in1=tmpB[:], op=ALU.add), [tmpA_T, tmpB_T], [tmpA_T])
                k.op('dve', lambda e, oc=oc: e.tensor_tensor(out=aT[:, 16 + oc, :], in0=tmpA[:], in1=tmpC[:], op=ALU.add), [tmpA_T, tmpC_T], [mg_T])
            for og in range(4):
                slot, sT = wl('O', l, og)
                for j in range(4):
                    oc = 4 * og + j
                    pT_, p_ = PS()
                    w = slot[:, j * 2048:(j + 1) * 2048]
                    k.mm(pT_, p_[:], [(w[:, kc * 128:(kc + 1) * 128], aT[:, 16 + kc, :]) for kc in range(KC)], [sT, mg_T])
                    evac_y(pT_, p_, oc, oc == 0, oc == 15)
            post_norm_residual(GG1)
            make_h(G2, 48)
            for ug in range(22):
                slot, sT = wl('U', l, ug)
                for pi in range(2):
                    j = 2 * ug + pi
                    accs = []
                    for gv in range(2):
                        ci = 2 * j + gv
                        pT_, p_ = proj_fm(slot, sT, 2 * pi + gv, hT, hT_T, KC, 0)
                        Zb, Zb_T = zb[ci % 2], zb_T[ci % 2]
                        k.op('act', lambda e, Zb=Zb, ci=ci: e.activation(out=Zb[:, 0:2], in_=carry[:, ci, :], func=AF.Copy), [carry_T], [Zb_T])
                        k.op('act', lambda e, Zb=Zb, p_=p_: e.activation(out=Zb[:, 2:2 + TT], in_=p_[:], func=AF.Copy), [pT_], [Zb_T])
                        k.op('act', lambda e, Zb=Zb, ci=ci: e.activation(out=carry[:, ci, :], in_=Zb[:, TT:TT + 2], func=AF.Copy), [Zb_T], [carry_T])
                        ac, ac_T = [(tmpA, tmpA_T), (tmpB, tmpB_T)][gv]
                        k.op('dve', lambda e, Zb=Zb, ac=ac, ci=ci: e.tensor_scalar(out=ac[:], in0=Zb[:, 2:2 + TT], scalar1=cw[:, 3 * ci + 2:3 * ci + 3], scalar2=cb[:, ci:ci + 1], op0=ALU.mult, op1=ALU.add), [Zb_T, LP], [ac_T])
                        k.op('dve', lambda e, Zb=Zb, ac=ac, ci=ci: e.scalar_tensor_tensor(out=ac[:], in0=Zb[:, 1:1 + TT], scalar=cw[:, 3 * ci + 1:3 * ci + 2], in1=ac[:], op0=ALU.mult, op1=ALU.add), [Zb_T, LP, ac_T], [ac_T])
                        k.op('dve', lambda e, Zb=Zb, ac=ac, ci=ci: e.scalar_tensor_tensor(out=ac[:], in0=Zb[:, 0:TT], scalar=cw[:, 3 * ci:3 * ci + 1], in1=ac[:], op0=ALU.mult, op1=ALU.add), [Zb_T, LP, ac_T], [ac_T])
                    k.op('act', lambda e: e.activation(out=tmpA[:], in_=tmpA[:], func=GELU), [tmpA_T], [tmpA_T])
                    k.op('dve', lambda e, j=j: e.tensor_tensor(out=aT[:, j, :], in0=tmpA[:], in1=tmpB[:], op=ALU.mult), [tmpA_T, tmpB_T], [aT_T])
            for oc in range(16):
                slot, sT = wl('D', l, oc)
                pT_, p_ = PS()
                k.mm(pT_, p_[:], [(slot[:, kc * 128:(kc + 1) * 128], aT[:, kc, :]) for kc in range(FC)], [sT, aT_T])
                evac_y(pT_, p_, oc, oc == 0, oc == 15)
            post_norm_residual(GG2)
            for c in range(KC):
                k.dma('sp', Xdst[c, :, c0:c0 + TT], xT[:, c, :], [xT_T], [XdstT[ti]])
            for t_ in (gu_T, ys_T, ym_T, yp_T, mg_T, vn_T, p4_T, pl_T):
                t_.r.update(aT_T.r); t_.r.update(aT_T.w)
    k.barrier()
    return nc, es


def _fm(v):
    n = v.shape[-1] // 128
    return np.ascontiguousarray(v.reshape(n, 128).T)


def _tile_w(W):
    K, N = W.shape
    nk, nj = K // 128, N // 128
    return np.ascontiguousarray(W.reshape(nk, 128, nj, 128).transpose(2, 1, 0, 3)).reshape(nj, 128, nk * 128)


def _prep_shared(inp, SEQ, DEPTH):
    f32 = np.float32
    bf = ml_dtypes.bfloat16
    L = DEPTH
    m = {}
    col_order = np.concatenate([np.concatenate([np.arange(j * 128, (j + 1) * 128), DFF + np.arange(j * 128, (j + 1) * 128)]) for j in range(FC)])
    gl = lambda n: np.asarray(inp[n], dtype=f32)
    m["gn"] = np.stack([np.concatenate([_fm(gl(n)[l]) for n in ("g_pre_mix", "g_post_mix", "g_pre_ffn", "g_post_ffn")], axis=1) for l in range(L)])
    m["bada"] = np.stack([_fm(gl("b_ada")[l]) for l in range(L)])
    m["binT"] = np.stack([_fm(gl("b_in")[l]) for l in range(L)])
    bc = lambda v: np.ascontiguousarray(np.broadcast_to(v[None, :], (128, v.shape[0])))
    m["bvB"] = np.stack([bc(gl("b_in")[l][512:1024]) for l in range(L)])
    m["bvaB"] = np.stack([bc(gl("b_in")[l][2048:2560]) for l in range(L)])
    m["sngB"] = np.stack([bc(gl("sgu_norm_g")[l]) for l in range(L)])
    sb_ = gl("sgu_b")
    m["sbT"] = np.ascontiguousarray(np.stack([np.stack([np.concatenate([np.broadcast_to(sb_[l, 2 * cc][None], (64, 128)), np.broadcast_to(sb_[l, 2 * cc + 1][None], (64, 128))], 0) for cc in range(4)], 1) for l in range(L)]))
    m["swT"] = np.ascontiguousarray(gl("sgu_w").transpose(0, 3, 1, 2))
    m["pw"] = np.ascontiguousarray(gl("pool_w").transpose(0, 2, 1, 3))
    m["psc"] = np.stack([_fm(gl("pool_scale")[l]) for l in range(L)])
    wc = gl("w_conv")[:, :, col_order]
    m["cw"] = np.ascontiguousarray(wc.reshape(L, 3, 88, 128).transpose(0, 3, 2, 1)).reshape(L, 128, 264)
    m["cb"] = np.stack([_fm(gl("b_conv")[l][col_order]) for l in range(L)])
    m["WA"] = np.stack([_tile_w(gl("w_ada")[l]).reshape(48, 2, 128, 2048).transpose(0, 2, 1, 3).reshape(48, 128, 4096) for l in range(L)])
    WI, WM, WO, WU, WD = [], [], [], [], []
    for l in range(L):
        tw = _tile_w(gl("w_in")[l])
        order = [0, 8, 12, 16, 4, 20]
        WI.append(np.stack([tw[o:o + 4].transpose(1, 0, 2).reshape(128, 8192) for o in order]))
        brs = [_tile_w(gl(n)[l]) for n in ("w_sgu_out", "w_moba_out", "w_pool_out")]
        WM.append(np.stack([np.concatenate([tw[24 + br * 16 + oc] for br in range(3)] + [brs[br][oc] for br in range(3)], axis=1) for oc in range(16)]))
        two = _tile_w(gl("w_out")[l])
        WO.append(np.stack([two[4 * g:4 * g + 4].transpose(1, 0, 2).reshape(128, 8192) for g in range(4)]))
        twu = _tile_w(gl("w_up")[l][:, col_order])
        WU.append(np.stack([twu[4 * g:4 * g + 4].transpose(1, 0, 2).reshape(128, 8192) for g in range(22)]))
        WD.append(_tile_w(gl("w_down")[l]))
    m["WI"], m["WM"], m["WO"], m["WU"], m["WD"] = (np.ascontiguousarray(np.stack(a)) for a in (WI, WM, WO, WU, WD))
    pos = np.arange(SEQ)
    kaux = np.zeros((8, 36, SEQ), f32); qaux = np.zeros((8, 4, SEQ), f32)
    for h in range(8):
        f = 2.0 ** (2 - h)
        for n in range(NBP):
            kaux[h, n] = (pos // 256 == n)
        kaux[h, 32] = f * (pos // 32 * 32); kaux[h, 33] = f * (pos % 32); kaux[h, 34:36] = 1.0
        qaux[h, 0:2] = 1.0; qaux[h, 2] = -f * (pos // 32 * 32); qaux[h, 3] = -f * (pos % 32)
    m["kaux"] = kaux.astype(bf); m["qaux"] = qaux.astype(bf)
    n_ = np.arange(NBP)
    m["blkb"] = np.ascontiguousarray(np.broadcast_to(np.where(n_[None, :] < n_[:, None], 0.0, -1e30).astype(f32)[None], (128, NBP, NBP)))
    m["owni"] = np.ascontiguousarray(np.broadcast_to((n_[None, :] == n_[:, None]).astype(f32)[None], (128, NBP, NBP)))
    p = np.arange(128)
    xx = np.arange(512)
    m["cm"] = np.stack([(xx[None, :] >= 128 * j + p[:, None]) for j in range(4)], 1).astype(f32).astype(bf)
    m["onesb"] = np.ones((128, 128), f32).astype(bf)
    m["ident"] = np.eye(128, dtype=f32).astype(bf)
    sh = np.zeros((128, 64), f32); sh[64 + np.arange(64), np.arange(64)] = 1.0
    m["shsel"] = sh.astype(bf)
    m["invc"] = np.ascontiguousarray(np.broadcast_to(np.stack([1.0 / np.minimum(xx + 1, w) for w in (2, 4, 8, 16)], 0).astype(f32)[None], (128, 4, 512)))
    m["triu"] = (p[:, None] <= p[None, :]).astype(f32)
    return m


def run(inputs, SEQ, DEPTH, n_cores=8):
    nc, es = build(SEQ, DEPTH)
    shared = _prep_shared(inputs, SEQ, DEPTH)
    x = np.asarray(inputs["x"], dtype=np.float32)
    c = np.asarray(inputs["c"], dtype=np.float32)
    B = x.shape[0]
    in_maps = []
    for core in range(n_cores):
        b = core % B
        mm_ = dict(shared)
        mm_["xT"] = np.ascontiguousarray(x[b].T).reshape(KC, 128, SEQ)
        mm_["cT"] = np.ascontiguousarray(c[b].reshape(KC, 128).T)
        in_maps.append(mm_)
    res = run_bass_kernel_spmd(nc, in_maps, core_ids=list(range(n_cores)))
    out = np.stack([np.ascontiguousarray(res.results[b]["outT"].reshape(D, SEQ).T) for b in range(B)])
    es.close()
    return out.astype(np.float32)


def kernel(**inputs):
    return run(inputs, 8192, 2, 8)
```

```python
import os
import numpy as np
import ml_dtypes
from contextlib import ExitStack
import concourse.bass as bass
import concourse.mybir as mybir
from concourse.bass_utils import run_bass_kernel_spmd

F32, BF16 = mybir.dt.float32, mybir.dt.bfloat16
AF = mybir.ActivationFunctionType
ALU = mybir.AluOpType
AX = mybir.AxisListType

D = 2048
KC = 16
TT = 512
DFF = 5632
FC = 44
NBP = 32
EPS = 1e-6
BIGZ = 240000.0
GELU = AF.Gelu_apprx_tanh


class T:
    def __init__(s, name, dg=None):
        s.name = name; s.w = {}; s.r = {}; s.dg = dg


class DS:
    def __init__(s, h):
        s.h = h; s.val = 0


class KB:
    def __init__(s, nc, es):
        s.nc = nc; s.es = es
        s.E = {'pe': nc.tensor, 'act': nc.scalar, 'dve': nc.vector, 'pool': nc.gpsimd, 'sp': nc.sync}
        s.sem = {e: es.enter_context(nc.semaphore('s_' + e)) for e in s.E}
        s.cnt = {e: 0 for e in s.E}
        s.seen = {e: {} for e in s.E}
        s.dsems = {}
        s.psi = 0

    def _wait(s, e, evs):
        for key, val in evs.items():
            if key == ('e', e) and e == 'pe':
                continue
            if key[0] == 'd':
                val = s.dsems[key[1]].val
            if s.seen[e].get(key, 0) >= val:
                continue
            h = s.sem[key[1]] if key[0] == 'e' else s.dsems[key[1]].h
            s.E[e].wait_ge(h, val)
            s.seen[e][key] = val

    def _deps(s, e, r, w):
        evs = {}
        def add(d):
            for k, v in d.items():
                if evs.get(k, 0) < v:
                    evs[k] = v
        for t in r:
            add(t.w)
            if getattr(t, 'psum', False):
                add(t.r)
        for t in w:
            add(t.w); add(t.r)
        s._wait(e, evs)

    def _rec(s, key, val, r, w):
        for t in r:
            t.r[key] = max(t.r.get(key, 0), val)
        for t in w:
            t.w = {key: val}; t.r = {}

    def op(s, e, fn, r, w):
        s._deps(e, r, w)
        inst = fn(s.E[e])
        s.cnt[e] += 1
        inst.then_inc(s.sem[e], 1)
        s._rec(('e', e), s.cnt[e], r, w)

    def mm(s, ps, out_ap, pairs, r, start=True, stop=True):
        s._deps('pe', r, [ps])
        n = len(pairs)
        inst = None
        for i, (a, b) in enumerate(pairs):
            inst = s.nc.tensor.matmul(out_ap, a, b, start=(start and i == 0), stop=(stop and i == n - 1))
        s.cnt['pe'] += 1
        inst.then_inc(s.sem['pe'], 1)
        s._rec(('e', 'pe'), s.cnt['pe'], r, [ps])

    def tr(s, ps, out_ap, in_ap, ident_ap, r):
        s._deps('pe', r, [ps])
        inst = s.nc.tensor.transpose(out_ap, in_ap, ident_ap)
        s.cnt['pe'] += 1
        inst.then_inc(s.sem['pe'], 1)
        s._rec(('e', 'pe'), s.cnt['pe'], r, [ps])

    def dma(s, q, out_ap, in_ap, r, w, **kw):
        s._deps(q, r, w)
        g = (w[0].dg or w[0].name) + '_' + q
        fifo = s.__dict__.setdefault('fifo_' + q, [])
        if len(fifo) >= 6:
            s._wait(q, {('d', fifo.pop(0)): 0})
        fifo.append(g)
        if g not in s.dsems:
            s.dsems[g] = DS(s.es.enter_context(s.nc.semaphore('d_' + g)))
        ds = s.dsems[g]
        s.E[q].dma_start(out=out_ap, in_=in_ap, **kw).then_inc(ds.h, 16)
        ds.val += 16
        s._rec(('d', g), ds.val, r, w)

    def barrier(s):
        evs = {('e', e): s.cnt[e] for e in s.E if s.cnt[e] > 0}
        evs.update({('d', g): ds.val for g, ds in s.dsems.items() if ds.val > 0})
        for e in s.E:
            s._wait(e, evs)


def build(SEQ, DEPTH):
    NT = SEQ // TT
    NKT = SEQ // 128
    nc = bass.Bass("TRN2", target_bir_lowering=False)
    es = ExitStack()
    k = KB(nc, es)

    def din(name, shape, dt=F32):
        return nc.dram_tensor(name, list(shape), dt, kind="ExternalInput").ap()

    def dscr(name, shape, dt):
        return nc.dram_tensor(name, list(shape), dt).ap()

    def sb(name, shape, dt=F32):
        return es.enter_context(nc.sbuf_tensor("sb_" + name, list(shape), dt))

    xT_in = din("xT", [KC, 128, SEQ])
    outT = nc.dram_tensor("outT", [KC, 128, SEQ], F32, kind="ExternalOutput").ap()
    cT_d = din("cT", [128, KC])
    gn_d = din("gn", [DEPTH, 128, 4 * KC])
    bada_d = din("bada", [DEPTH, 128, 96])
    bin_d = din("binT", [DEPTH, 128, 72])
    bvB_d = din("bvB", [DEPTH, 128, 512])
    bvaB_d = din("bvaB", [DEPTH, 128, 512])
    sngB_d = din("sngB", [DEPTH, 128, 512])
    sbT_d = din("sbT", [DEPTH, 128, 4, 128])
    swT_d = din("swT", [DEPTH, 128, 8, 128])
    pw_d = din("pw", [DEPTH, 128, 4, 128])
    psc_d = din("psc", [DEPTH, 128, 4])
    cw_d = din("cw", [DEPTH, 128, 88 * 3])
    cb_d = din("cb", [DEPTH, 128, 88])
    WA_d = din("WA", [DEPTH, 48, 128, 2 * KC * 128])
    WI_d = din("WI", [DEPTH, 6, 128, 4 * KC * 128])
    WM_d = din("WM", [DEPTH, 16, 128, 7680])
    WO_d = din("WO", [DEPTH, 4, 128, 4 * KC * 128])
    WU_d = din("WU", [DEPTH, 22, 128, 4 * KC * 128])
    WD_d = din("WD", [DEPTH, 16, 128, FC * 128])
    kaux_d = din("kaux", [8, 36, SEQ], BF16)
    qaux_d = din("qaux", [8, 4, SEQ], BF16)
    blkb_d = din("blkb", [128, NBP, NBP])
    owni_d = din("owni", [128, NBP, NBP])
    cm_d = din("cm", [128, 4, 512], BF16)
    ones_d = din("onesb", [128, 128], BF16)
    ident_d = din("ident", [128, 128], BF16)
    shsel_d = din("shsel", [128, 64], BF16)
    invc_d = din("invc", [128, 4, 512])
    triu_d = din("triu", [128, 128])

    Xs = dscr("Xs", [KC, 128, SEQ], F32)
    QT16 = dscr("QT16", [4, 128, SEQ], BF16)
    KT16 = dscr("KT16", [4, 128, SEQ], BF16)
    V16 = dscr("V16", [NKT, 128, 512], BF16)
    MB = dscr("MB", [8, NBP, SEQ], BF16)
    YM = dscr("YM", [4, 128, SEQ], BF16)

    XinT = [T("xin%d" % i, dg="xin") for i in range(NT)]
    XsT = [T("xs%d" % i, dg="xsd") for i in range(NT)]
    XoT = [T("xo%d" % i, dg="xod") for i in range(NT)]
    QT16t, KT16t, V16t, MBt, YMt = T("QT16"), T("KT16"), T("V16"), T("MB"), T("YM")
    CONST = T("const")

    ps_t = [es.enter_context(nc.psum_tensor("ps%d" % i, [128, 512], F32)) for i in range(8)]
    ps_T = [T("ps%d" % i) for i in range(8)]
    for t_ in ps_T:
        t_.psum = True

    def PS():
        i = k.psi; k.psi = (k.psi + 1) % 6
        return ps_T[i], ps_t[i]
    ACC_T, ACC = ps_T[6], ps_t[6]
    AUX_T, AUX = ps_T[7], ps_t[7]

    onesb = sb("onesb", [128, 128], BF16)
    ident = sb("ident", [128, 128], BF16)
    shsel = sb("shsel", [128, 64], BF16)
    triu = sb("triu", [128, 128])
    cm = sb("cm", [128, 4, 512], BF16)
    blkb = sb("blkb", [128, 2, NBP]); owni = sb("owni", [128, 2, NBP]); BO_T = T("blkown")
    cT = sb("cT", [128, KC])
    condT = sb("condT", [128, KC], BF16)
    cst = [(onesb, ones_d), (ident, ident_d), (shsel, shsel_d), (triu, triu_d), (cm, cm_d),
           (cT, cT_d)]
    for t_, d_ in cst:
        k.dma('sp', t_[:], d_, [], [CONST])
    k.op('act', lambda e: e.activation(out=condT[:], in_=cT[:], func=AF.Silu), [CONST], [CONST])

    gn = sb("gn", [128, 4 * KC]); bada = sb("bada", [128, 96]); binT = sb("binT", [128, 72])
    bvB = sb("bvB", [128, 512]); bvaB = sb("bvaB", [128, 512]); sngB = sb("sngB", [128, 512])
    sbT = sb("sbT", [128, 4, 128]); swm = sb("swm", [128, 8, 128], BF16)
    pwb = sb("pwb", [128, 4, 128], BF16); psc = sb("psc", [128, 4])
    cw = sb("cw", [128, 88 * 3]); cb = sb("cb", [128, 88])
    modT = sb("modT", [128, 96])
    G1 = sb("G1", [128, KC]); GG1 = sb("GG1", [128, KC]); G2 = sb("G2", [128, KC]); GG2 = sb("GG2", [128, KC])
    kmT = sb("kmT", [128, 4, NBP])
    LP = T("lparams")

    NSLOT = 3
    slots = [sb("slot%d" % i, [128, 8192], BF16) for i in range(NSLOT)]
    slotT = [T("slot%d" % i) for i in range(NSLOT)]
    sl = {'i': 0}

    def wload(src_ap, nelem, cast=True):
        i = sl['i']; sl['i'] = (i + 1) % NSLOT
        if cast and not os.environ.get('KNOW'):
            k.dma('pool', slots[i][:, 0:nelem], src_ap, [], [slotT[i]], max_dma_last_dim=4096)
        return slots[i], slotT[i]

    xTf = sb("xTt", [128, KC * TT]); xT_T = T("xTt")
    xT = xTf[:].rearrange("p (c t) -> p c t", c=KC)
    XB = xTf[:].bitcast(BF16)
    hT = sb("hTt", [128, KC, TT], BF16); hT_T = T("hTt")
    yT = sb("yTt", [128, KC, TT], BF16); yT_T = T("yTt")
    aT = sb("aTt", [128, FC, TT], BF16); aT_T = T("aTt")
    tmpAx = sb("tmpA", [128, TT + 16]); tmpA_T = T("tmpA")
    tmpBx = sb("tmpB", [128, TT + 16]); tmpB_T = T("tmpB")
    tmpA = tmpAx[:, 0:TT]; tmpB = tmpBx[:, 0:TT]
    tmpC = sb("tmpC", [128, TT]); tmpC_T = T("tmpC")
    tmpD = sb("tmpD", [128, TT]); tmpD_T = T("tmpD")
    sq = [sb("sq%d" % i, [128, TT], BF16) for i in range(2)]; sq_T = [T("sq%d" % i) for i in range(2)]
    rstdB = sb("rstdB", [128, TT]); rstdB_T = T("rstdB")
    st6 = sb("st6", [128, 6]); mv2 = sb("mv2", [128, 2]); sm_T = T("small")
    zb = [sb("zb%d" % i, [128, TT + 2]) for i in range(2)]; zb_T = [T("zb%d" % i) for i in range(2)]
    carry = sb("carry", [128, 88, 2]); carry_T = T("carry")
    zp = [sb("zp%d" % i, [128, TT + 16]) for i in range(2)]; zp_T = [T("zp%d" % i) for i in range(2)]
    halo = sb("halo", [128, 4, 16]); halo_T = T("halo")
    pa, pa_T, pb, pb_T = tmpAx, tmpA_T, tmpBx, tmpB_T
    pbufT = [T("pbuf%d" % i) for i in range(4)]
    KM_T = T("kmT")
    kmH = sb("kmH", [128, 4, NBP], BF16); kmL = sb("kmL", [128, 4, NBP], BF16)
    qlo = sb("qlo", [128, TT], BF16); qlo_T = T("qlo")
    mbb = sb("mbb", [128, 4 * NBP], BF16); mbb_T = T("mbb")
    hl_T = T("hl")

    guT = aT[:, 0:4, :]; ysT = aT[:, 4:8, :]; ymT = aT[:, 8:12, :]; ypT = aT[:, 12:16, :]
    mgT = aT[:, 16:32, :]; vnT = aT[:, 32:36, :]; pT4 = aT[:, 36:40, :]; plT = aT[:, 40:44, :]
    gu_T, ys_T, ym_T, yp_T, mg_T, vn_T, p4_T, pl_T = (T("gu"), T("ys"), T("ym"), T("yp"), T("mg"), T("vn"), T("p4"), T("pl"))
    KTa = XB[:, 0:SEQ]; KTa_T = T("KTa")
    Va = XB[:, 8192:8192 + NKT * 128].rearrange("p (k c) -> p k c", c=128); Va_T = T("Va")
    QTa = [sb("QTa%d" % i, [128, TT], BF16) for i in range(2)]; QTa_T = [T("QTa%d" % i) for i in range(2)]

    def stats_rstd(src_tile, src_T, nch):
        for c in range(nch):
            j = c % 2
            k.op('act', lambda e, c=c, j=j: e.activation(out=sq[j][:], in_=src_tile[:, c, :], func=AF.Square), [src_T], [sq_T[j]])
            k.mm(ACC_T, ACC[:], [(onesb[:], sq[j][:])], [sq_T[j], CONST], start=(c == 0), stop=(c == nch - 1))
        k.op('dve', lambda e: e.tensor_scalar(out=tmpD[:], in0=ACC[:], scalar1=1.0 / D, scalar2=EPS, op0=ALU.mult, op1=ALU.add), [ACC_T], [tmpD_T])
        k.op('act', lambda e: e.activation(out=tmpD[:], in_=tmpD[:], func=AF.Sqrt), [tmpD_T], [tmpD_T])
        k.op('dve', lambda e: e.reciprocal(out=rstdB[:], in_=tmpD[:]), [tmpD_T], [rstdB_T])

    def make_h(Gs, SHcol):
        stats_rstd(xT, xT_T, KC)
        for c in range(KC):
            tt, tT = (tmpA, tmpA_T) if c % 2 == 0 else (tmpB, tmpB_T)
            k.op('dve', lambda e, c=c, tt=tt: e.scalar_tensor_tensor(out=tt[:], in0=xT[:, c, :], scalar=Gs[:, c:c + 1], in1=rstdB[:], op0=ALU.mult, op1=ALU.mult), [xT_T, rstdB_T, LP], [tT])
            k.op('act', lambda e, c=c, tt=tt: e.activation(out=hT[:, c, :], in_=tt[:], func=AF.Identity, bias=modT[:, SHcol + c:SHcol + c + 1], scale=1.0), [tT, LP], [hT_T])

    def proj_fm(slot, sT, j, rhs_tile, rhs_T, nk, kstride):
        pT_, p_ = PS()
        w = slot[:, j * nk * 128:(j + 1) * nk * 128]
        k.mm(pT_, p_[:], [(w[:, kc * 128:(kc + 1) * 128], rhs_tile[:, kc, :]) for kc in range(nk)], [sT, rhs_T])
        return pT_, p_

    def post_norm_residual(GGs):
        k.op('dve', lambda e: e.tensor_scalar(out=tmpD[:], in0=ACC[:], scalar1=1.0 / D, scalar2=EPS, op0=ALU.mult, op1=ALU.add), [ACC_T], [tmpD_T])
        k.op('act', lambda e: e.activation(out=tmpD[:], in_=tmpD[:], func=AF.Sqrt), [tmpD_T], [tmpD_T])
        k.op('dve', lambda e: e.reciprocal(out=rstdB[:], in_=tmpD[:]), [tmpD_T], [rstdB_T])
        for c in range(KC):
            tt, tT = (tmpA, tmpA_T) if c % 2 == 0 else (tmpB, tmpB_T)
            k.op('dve', lambda e, c=c, tt=tt: e.scalar_tensor_tensor(out=tt[:], in0=yT[:, c, :], scalar=GGs[:, c:c + 1], in1=rstdB[:], op0=ALU.mult, op1=ALU.mult), [yT_T, rstdB_T, LP], [tT])
            k.op('dve', lambda e, c=c, tt=tt: e.tensor_tensor(out=xT[:, c, :], in0=xT[:, c, :], in1=tt[:], op=ALU.add), [tT, xT_T], [xT_T])

    def evac_y(pT_, p_, oc, first, last):
        k.op('act', lambda e: e.activation(out=yT[:, oc, :], in_=p_[:], func=AF.Copy), [pT_], [yT_T])
        j = oc % 2
        k.op('act', lambda e: e.activation(out=sq[j][:], in_=p_[:], func=AF.Square), [pT_], [sq_T[j]])
        k.mm(ACC_T, ACC[:], [(onesb[:], sq[j][:])], [sq_T[j], CONST], start=first, stop=last)

    for l in range(DEPTH):
        Xsrc, XsrcT = (xT_in, XinT) if l == 0 else (Xs, XsT)
        Xdst, XdstT = (outT, XoT) if l == DEPTH - 1 else (Xs, XsT)
        k.barrier()
        for t_, d_ in [(gn, gn_d), (bada, bada_d), (binT, bin_d), (bvB, bvB_d), (bvaB, bvaB_d), (sngB, sngB_d),
                       (sbT, sbT_d), (psc, psc_d), (cw, cw_d), (cb, cb_d)]:
            k.dma('sp', t_[:], d_[l], [], [LP])
        k.dma('pool', pwb[:], pw_d[l], [], [LP])
        for hf in range(2):
            k.dma('sp', tmpA[:].rearrange('p (g t) -> p g t', g=4), swT_d[l][:, 4 * hf:4 * hf + 4, :], [], [tmpA_T])
            for g4 in range(4):
                k.op('dve', lambda e, g4=g4, hf=hf: e.tensor_tensor(out=swm[:, 4 * hf + g4, :], in0=tmpA[:, g4 * 128:(g4 + 1) * 128], in1=triu[:], op=ALU.mult), [tmpA_T, CONST], [LP])
        k.op('dve', lambda e: e.memset(kmT[:], 0.0), [], [KM_T])
        k.op('dve', lambda e: e.memset(kmH[:], 0.0), [KM_T], [KM_T])
        k.op('dve', lambda e: e.memset(kmL[:], 0.0), [KM_T], [KM_T])
        k.op('dve', lambda e: e.memset(carry[:], 0.0), [], [carry_T])
        k.op('dve', lambda e: e.memset(halo[:], 0.0), [], [halo_T])
        for jj in range(48):
            slot, sT = wload(WA_d[l, jj], 4096)
            for h2 in range(2):
                j = 2 * jj + h2
                k.mm(AUX_T, AUX[:, j:j + 1], [(slot[:, (h2 * KC + kc) * 128:(h2 * KC + kc + 1) * 128], condT[:, kc:kc + 1]) for kc in range(KC)], [sT, CONST])
        k.op('dve', lambda e: e.tensor_tensor(out=modT[:], in0=AUX[:, 0:96], in1=bada[:], op=ALU.add), [AUX_T, LP], [LP])
        k.op('dve', lambda e: e.scalar_tensor_tensor(out=G1[:], in0=modT[:, 16:32], scalar=1.0, in1=gn[:, 0:16], op0=ALU.add, op1=ALU.mult), [LP], [LP])
        k.op('dve', lambda e: e.tensor_tensor(out=GG1[:], in0=modT[:, 32:48], in1=gn[:, 16:32], op=ALU.mult), [LP], [LP])
        k.op('dve', lambda e: e.scalar_tensor_tensor(out=G2[:], in0=modT[:, 64:80], scalar=1.0, in1=gn[:, 32:48], op0=ALU.add, op1=ALU.mult), [LP], [LP])
        k.op('dve', lambda e: e.tensor_tensor(out=GG2[:], in0=modT[:, 80:96], in1=gn[:, 48:64], op=ALU.mult), [LP], [LP])

        if os.environ.get('KSTOP') == 'ada':
            k.barrier(); return nc, es
        for ti in range(NT):
            c0 = ti * TT
            for q4 in range(4):
                k.dma('sp', xT[:, 4 * q4:4 * q4 + 4, :], Xsrc[4 * q4:4 * q4 + 4, :, c0:c0 + TT].rearrange("c p t -> p c t"), [XsrcT[ti]], [xT_T])
            make_h(G1, 0)
            k.dma('sp', blkb[:], blkb_d[:, 2 * ti:2 * ti + 2, :], [], [BO_T])
            k.dma('sp', owni[:], owni_d[:, 2 * ti:2 * ti + 2, :], [], [BO_T])
            SECT = os.environ.get('KSECT', 'K,Q,QS,V,KD,QD,VD,MBD,TR').split(',')
            slot, sT = wload(WI_d[l, 2], 8192)
            for c in range(4 if 'K' in SECT else 0):
                pT_, p_ = proj_fm(slot, sT, c, hT, hT_T, KC, 0)
                bcol = binT[:, 12 + c:13 + c]
                k.op('act', lambda e, p_=p_, c=c, bcol=bcol: e.activation(out=aT[:, c, :], in_=p_[:], func=AF.Identity, bias=bcol, scale=1.0), [pT_, LP], [p4_T])
                k.op('dve', lambda e, p_=p_: e.tensor_reduce(out=mv2[:], in_=p_[:].rearrange("p (b s) -> p b s", s=256), axis=AX.X, op=ALU.add), [pT_], [sm_T])
                k.op('dve', lambda e: e.tensor_scalar(out=mv2[:], in0=mv2[:], scalar1=1.0 / 256, scalar2=None, op0=ALU.mult), [sm_T], [sm_T])
                k.op('dve', lambda e, c=c, bcol=bcol: e.tensor_scalar(out=kmT[:, c, 2 * ti:2 * ti + 2], in0=mv2[:], scalar1=bcol, scalar2=None, op0=ALU.add), [sm_T, LP], [KM_T])
                k.op('dve', lambda e, c=c: e.tensor_copy(out=kmH[:, c, :], in_=kmT[:, c, :]), [KM_T], [KM_T])
                k.op('dve', lambda e, c=c: e.tensor_tensor(out=kmL[:, c, :], in0=kmT[:, c, :], in1=kmH[:, c, :], op=ALU.subtract), [KM_T], [KM_T])
            if 'KD' in SECT:
                for c in range(4):
                    k.dma('sp', KT16[c, :, c0:c0 + TT], aT[:, c, :], [p4_T], [KT16t])
            slot, sT = wload(WI_d[l, 1], 8192)
            for c in range(4 if 'Q' in SECT else 0):
                pT_, p_ = proj_fm(slot, sT, c, hT, hT_T, KC, 0)
                bcol = binT[:, 8 + c:9 + c]
                k.op('act', lambda e, p_=p_, c=c, bcol=bcol: e.activation(out=aT[:, 4 + c, :], in_=p_[:], func=AF.Identity, bias=bcol, scale=1.0), [pT_, LP], [pl_T])
                k.op('dve', lambda e, p_=p_, bcol=bcol: e.tensor_scalar(out=tmpC[:], in0=p_[:], scalar1=bcol, scalar2=None, op0=ALU.add), [pT_, LP], [tmpC_T])
                k.op('dve', lambda e, c=c: e.tensor_tensor(out=qlo[:], in0=tmpC[:], in1=aT[:, 4 + c, :], op=ALU.subtract), [tmpC_T, pl_T], [qlo_T])
                for hh in range(2 if 'QS' in SECT else 0):
                    h = 2 * c + hh
                    r0 = 64 * hh
                    sT_, s_ = PS()
                    for su in range(4):
                        qh = aT[r0:r0 + 64, 4 + c, su * 128:(su + 1) * 128]; ql = qlo[r0:r0 + 64, su * 128:(su + 1) * 128]
                        k.mm(sT_, s_[:, su * NBP:(su + 1) * NBP], [(qh, kmH[r0:r0 + 64, c, :]), (ql, kmH[r0:r0 + 64, c, :]), (qh, kmL[r0:r0 + 64, c, :])], [qlo_T, pl_T, KM_T])
                    for su in range(4):
                        k.op('dve', lambda e, s_=s_, su=su: e.tensor_tensor(out=tmpA[:, su * NBP:(su + 1) * NBP], in0=s_[:, su * NBP:(su + 1) * NBP], in1=blkb[:, su // 2, :], op=ALU.add), [sT_, BO_T], [tmpA_T])
                    for su in range(4):
                        seg = tmpA[:, su * NBP:(su + 1) * NBP]
                        k.op('dve', lambda e, seg=seg: e.max(out=tmpB[:, 0:8], in_=seg), [tmpA_T], [tmpB_T])
                        k.op('dve', lambda e: e.tensor_scalar(out=tmpB[:, 8:9], in0=tmpB[:, 2:3], scalar1=-1e29, scalar2=None, op0=ALU.max), [tmpB_T], [tmpB_T])
                        k.op('dve', lambda e, seg=seg: e.tensor_scalar(out=seg, in0=seg, scalar1=tmpB[:, 8:9], scalar2=None, op0=ALU.is_ge), [tmpB_T, tmpA_T], [tmpA_T])
                        k.op('dve', lambda e, seg=seg, su=su: e.tensor_tensor(out=seg, in0=seg, in1=owni[:, su // 2, :], op=ALU.max), [tmpA_T, BO_T], [tmpA_T])
                        k.op('dve', lambda e, seg=seg, su=su: e.tensor_scalar(out=mbb[:, su * NBP:(su + 1) * NBP], in0=seg, scalar1=-1.0, scalar2=BIGZ, op0=ALU.add, op1=ALU.mult), [tmpA_T], [mbb_T])
                    if 'TR' not in SECT:
                        continue
                    tT_, t_ = PS()
                    for su in range(4):
                        k.tr(tT_, t_[:].bitcast(BF16)[0:NBP, su * 128:(su + 1) * 128], mbb[:, su * NBP:(su + 1) * NBP], ident[:], [mbb_T, CONST])
                    j = h % 2
                    k.op('act', lambda e, t_=t_, j=j: e.activation(out=sq[j][0:NBP, :], in_=t_[:].bitcast(BF16)[0:NBP, 0:TT], func=AF.Copy), [tT_], [sq_T[j]])
                    if 'MBD' in SECT:
                        k.dma('sp', MB[h, :, c0:c0 + TT], sq[j][0:NBP, :], [sq_T[j]], [MBt])
            if 'QD' in SECT:
                for c in range(4):
                    k.dma('sp', QT16[c, :, c0:c0 + TT], aT[:, 4 + c, :], [pl_T], [QT16t])
            slot, sT = wload(WI_d[l, 3], 8192)
            wv = slot[:].rearrange("p (j k c) -> p j k c", j=4, k=KC)
            for su in range(4 if 'V' in SECT else 0):
                pT_, p_ = PS()
                k.mm(pT_, p_[:].rearrange("p (j c) -> p j c", j=4), [(hT[:, kc, su * 128:(su + 1) * 128], wv[:, :, kc, :]) for kc in range(KC)], [sT, hT_T])
                k.op('dve', lambda e, p_=p_, su=su: e.tensor_tensor(out=aT[:, 8 + su, :], in0=p_[:], in1=bvaB[:], op=ALU.add), [pT_, LP], [ym_T])
                kt = ti * 4 + su
                if 'VD' not in SECT:
                    continue
                k.dma('sp', V16[kt], aT[:, 8 + su, :], [ym_T], [V16t])

        if os.environ.get('KSTOP') == 'A':
            k.barrier(); return nc, es
        k.barrier()
        k.op('dve', lambda e: e.memset(Va[:, :, 64:128], 1.0), [], [Va_T])
        for h in range(8):
            c, r0 = h // 2, 64 * (h % 2)
            k.dma('sp', KTa[0:64, :], KT16[c, r0:r0 + 64, :], [KT16t], [KTa_T])
            k.dma('sp', KTa[64:100, :], kaux_d[h], [], [KTa_T])
            for k8 in range(0, NKT, 8):
                k.dma('sp', Va[:, k8:k8 + 8, 0:64], V16[k8:k8 + 8, :, h * 64:(h + 1) * 64].rearrange("k p d -> p k d"), [V16t], [Va_T])
            for ti in range(NT):
                c0 = ti * TT
                qi = (h * NT + ti) % 2
                Q, Q_T = QTa[qi], QTa_T[qi]
                k.dma('sp', Q[0:64, :], QT16[c, r0:r0 + 64, c0:c0 + TT], [QT16t], [Q_T])
                k.dma('sp', Q[64:96, :], MB[h, :, c0:c0 + TT], [MBt], [Q_T])
                k.dma('sp', Q[96:100, :], qaux_d[h, :, c0:c0 + TT], [], [Q_T])
                nkt = 4 * ti + 4
                for kt in range(nkt):
                    sT_, s_ = PS()
                    k.mm(sT_, s_[:], [(KTa[0:100, kt * 128:(kt + 1) * 128], Q[0:100, :])], [KTa_T, Q_T])
                    pj = kt % 4
                    P = hT[:, pj, :]
                    PTt = pbufT[pj]
                    if kt >= 4 * ti:
                        k.op('dve', lambda e, s_=s_: e.tensor_scalar(out=tmpC[:], in0=s_[:], scalar1=400.0, scalar2=None, op0=ALU.min), [sT_], [tmpC_T])
                        k.op('act', lambda e, P=P: e.activation(out=P, in_=tmpC[:], func=AF.Exp, scale=0.125), [tmpC_T], [PTt])
                    else:
                        k.op('act', lambda e, s_=s_, P=P: e.activation(out=P, in_=s_[:], func=AF.Exp, scale=0.125), [sT_], [PTt])
                    if kt >= 4 * ti:
                        k.op('dve', lambda e, P=P, kt=kt: e.tensor_tensor(out=P, in0=P, in1=cm[:, kt - 4 * ti, :], op=ALU.mult), [PTt, CONST], [PTt])
                    k.mm(ACC_T, ACC[:], [(Va[:, kt, :], P)], [Va_T, PTt], start=(kt == 0), stop=(kt == nkt - 1))
                k.op('act', lambda e: e.activation(out=tmpA[:], in_=ACC[:], func=AF.Copy), [ACC_T], [tmpA_T])
                k.op('dve', lambda e: e.tensor_copy(out=hT[:, 4, :], in_=tmpA[:]), [tmpA_T], [hl_T])
                k.op('dve', lambda e: e.tensor_tensor(out=hT[:, 5, :], in0=tmpA[:], in1=hT[:, 4, :], op=ALU.subtract), [tmpA_T, hl_T], [hl_T])
                k.mm(AUX_T, AUX[0:64, :], [(shsel[:], hT[:, 4, :]), (shsel[:], hT[:, 5, :])], [hl_T, CONST])
                k.op('dve', lambda e: e.reciprocal(out=tmpB[0:64, :], in_=AUX[0:64, :]), [AUX_T], [tmpB_T])
                j = ti % 2
                k.op('dve', lambda e, j=j: e.tensor_tensor(out=sq[j][0:64, :], in0=tmpA[0:64, :], in1=tmpB[0:64, :], op=ALU.mult), [tmpA_T, tmpB_T], [sq_T[j]])
                k.dma('sp', YM[c, r0:r0 + 64, c0:c0 + TT], sq[j][0:64, :], [sq_T[j]], [YMt])

        if os.environ.get('KSTOP') == 'B':
            k.barrier(); return nc, es
        k.barrier()
        for ti in range(NT):
            c0 = ti * TT
            for q4 in range(4):
                k.dma('sp', xT[:, 4 * q4:4 * q4 + 4, :], Xsrc[4 * q4:4 * q4 + 4, :, c0:c0 + TT].rearrange("c p t -> p c t"), [XsrcT[ti]], [xT_T])
            k.dma('sp', ymT, YM[:, :, c0:c0 + TT].rearrange("c p t -> p c t"), [YMt], [ym_T])
            make_h(G1, 0)
            slot, sT = wload(WI_d[l, 0], 8192)
            for c in range(4):
                pT_, p_ = proj_fm(slot, sT, c, hT, hT_T, KC, 0)
                k.op('act', lambda e, p_=p_, c=c: e.activation(out=aT[:, c, :], in_=p_[:], func=GELU, bias=binT[:, c:c + 1], scale=1.0), [pT_, LP], [gu_T])
            slot, sT = wload(WI_d[l, 4], 8192)
            wv = slot[:].rearrange("p (j k c) -> p j k c", j=4, k=KC)
            for su in range(4):
                pT_, p_ = PS()
                k.mm(pT_, p_[:].rearrange("p (j c) -> p j c", j=4), [(hT[:, kc, su * 128:(su + 1) * 128], wv[:, :, kc, :]) for kc in range(KC)], [sT, hT_T])
                k.op('dve', lambda e, p_=p_: e.tensor_tensor(out=tmpA[:], in0=p_[:], in1=bvB[:], op=ALU.add), [pT_, LP], [tmpA_T])
                k.op('act', lambda e: e.activation(out=tmpA[:], in_=tmpA[:], func=GELU), [tmpA_T], [tmpA_T])
                k.op('dve', lambda e: e.bn_stats(out=st6[:], in_=tmpA[:]), [tmpA_T], [sm_T])
                k.op('dve', lambda e: e.bn_aggr(out=mv2[:], in_=st6[:]), [sm_T], [sm_T])
                k.op('dve', lambda e: e.tensor_scalar(out=mv2[:, 1:2], in0=mv2[:, 1:2], scalar1=EPS, scalar2=None, op0=ALU.add), [sm_T], [sm_T])
                k.op('act', lambda e: e.activation(out=mv2[:, 1:2], in_=mv2[:, 1:2], func=AF.Sqrt), [sm_T], [sm_T])
                k.op('dve', lambda e: e.reciprocal(out=mv2[:, 1:2], in_=mv2[:, 1:2]), [sm_T], [sm_T])
                k.op('dve', lambda e: e.tensor_scalar(out=tmpA[:], in0=tmpA[:], scalar1=mv2[:, 0:1], scalar2=mv2[:, 1:2], op0=ALU.subtract, op1=ALU.mult), [sm_T, tmpA_T], [tmpA_T])
                k.op('dve', lambda e, su=su: e.tensor_tensor(out=aT[:, 32 + su, :], in0=tmpA[:], in1=sngB[:], op=ALU.mult), [tmpA_T, LP], [vn_T])
            for cc in range(4):
                pT_, p_ = PS()
                for su in range(4):
                    for gg in range(2):
                        g = 2 * cc + gg
                        k.mm(pT_, p_[64 * gg:64 * gg + 64, su * 128:(su + 1) * 128], [(aT[:, 32 + su, g * 64:(g + 1) * 64], swm[:, g, :])], [vn_T, LP])
                for su in range(4):
                    k.op('dve', lambda e, p_=p_, cc=cc, su=su: e.tensor_tensor(out=tmpA[:, su * 128:(su + 1) * 128], in0=p_[:, su * 128:(su + 1) * 128], in1=sbT[:, cc, :], op=ALU.add), [pT_, LP], [tmpA_T])
                k.op('dve', lambda e, cc=cc: e.tensor_tensor(out=aT[:, 4 + cc, :], in0=tmpA[:], in1=aT[:, cc, :], op=ALU.mult), [tmpA_T, gu_T], [ys_T])
            slot, sT = wload(WI_d[l, 5], 8192)
            for gi in range(4):
                w = (2, 4, 8, 16)[gi]
                pT_, p_ = proj_fm(slot, sT, gi, hT, hT_T, KC, 0)
                Z, Z_T = zp[gi % 2], zp_T[gi % 2]
                k.op('act', lambda e, Z=Z, gi=gi: e.activation(out=Z[:, 0:16], in_=halo[:, gi, :], func=AF.Copy), [halo_T], [Z_T])
                k.op('act', lambda e, p_=p_, Z=Z, gi=gi: e.activation(out=Z[:, 16:16 + TT], in_=p_[:], func=AF.Identity, bias=binT[:, 20 + gi:21 + gi], scale=1.0), [pT_, LP], [Z_T])
                bufs = [(pa, pa_T), (pb, pb_T)]
                src, src_T = Z, Z_T
                step = 1; bi = 0; lo = 0
                while step < w:
                    lo += step
                    dst, dst_T = bufs[bi]; bi ^= 1
                    k.op('dve', lambda e, dst=dst, src=src, lo=lo, step=step: e.tensor_tensor(out=dst[:, lo:TT + 16], in0=src[:, lo:TT + 16], in1=src[:, lo - step:TT + 16 - step], op=ALU.add), [src_T], [dst_T])
                    src, src_T = dst, dst_T
                    step *= 2
                if ti == 0:
                    k.dma('sp', tmpD[:], invc_d[:, gi, :], [], [tmpD_T])
                    k.op('dve', lambda e, src=src, gi=gi: e.tensor_tensor(out=tmpC[:], in0=src[:, 16:16 + TT], in1=tmpD[:], op=ALU.mult), [src_T, tmpD_T], [tmpC_T])
                    k.op('dve', lambda e, Z=Z, gi=gi: e.tensor_tensor(out=aT[:, 40 + gi, :], in0=tmpC[:], in1=Z[:, 16:16 + TT], op=ALU.subtract), [tmpC_T, Z_T], [pl_T])
                else:
                    k.op('dve', lambda e, src=src, Z=Z, gi=gi, w=w: e.scalar_tensor_tensor(out=aT[:, 40 + gi, :], in0=src[:, 16:16 + TT], scalar=1.0 / w, in1=Z[:, 16:16 + TT], op0=ALU.mult, op1=ALU.subtract), [src_T, Z_T], [pl_T])
                k.op('act', lambda e, Z=Z, gi=gi: e.activation(out=halo[:, gi, :], in_=Z[:, TT:TT + 16], func=AF.Copy), [Z_T], [halo_T])
                qT_, q_ = PS()
                k.mm(qT_, q_[:], [(pwb[:, gi, :], aT[:, 40 + gi, :])], [pl_T, LP])
                k.op('act', lambda e, q_=q_, gi=gi: e.activation(out=aT[:, 12 + gi, :], in_=q_[:], func=AF.Identity, scale=psc[:, gi:gi + 1]), [qT_, LP], [yp_T])
            brs = [(4, ys_T), (8, ym_T), (12, yp_T)]
            for oc in range(16):
                slot, sT = wload(WM_d[l, oc], 7680)
                for br in range(3):
                    gT_, g_ = PS()
                    wg = slot[:, br * 2048:(br + 1) * 2048]
                    k.mm(gT_, g_[:], [(wg[:, kc * 128:(kc + 1) * 128], hT[:, kc, :]) for kc in range(KC)], [sT, hT_T])
                    gcol = 24 + br * 16 + oc
                    gt_, gtT = [(tmpA, tmpA_T), (tmpB, tmpB_T), (tmpC, tmpC_T)][br]
                    k.op('act', lambda e, g_=g_, gt_=gt_, gcol=gcol: e.activation(out=gt_[:], in_=g_[:], func=AF.Sigmoid, bias=binT[:, gcol:gcol + 1], scale=1.0), [gT_, LP], [gtT])
                    bT_, b_ = PS()
                    wb = slot[:, 6144 + br * 512:6144 + (br + 1) * 512]
                    a0, aT_ = brs[br]
                    k.mm(bT_, b_[:], [(wb[:, kc * 128:(kc + 1) * 128], aT[:, a0 + kc, :]) for kc in range(4)], [sT, aT_])
                    k.op('dve', lambda e, b_=b_, gt_=gt_: e.tensor_tensor(out=gt_[:], in0=b_[:], in1=gt_[:], op=ALU.mult), [bT_, gtT], [gtT])
                k.op('dve', lambda e: e.tensor_tensor(out=tmpA[:], in0=tmpA[:], in1=tmpB[:], op=ALU.add), [tmpA_T, tmpB_T], [tmpA_T])
                k.op('dve', lambda e, oc=oc: e.tensor_tensor(out=aT[:, 16 + oc, :], in0=tmpA[:], in1=tmpC[:], op=ALU.add), [tmpA_T, tmpC_T], [mg_T])
            for og in range(4):
                slot, sT = wload(WO_d[l, og], 8192)
                for j in range(4):
                    oc = 4 * og + j
                    pT_, p_ = PS()
                    w = slot[:, j * 2048:(j + 1) * 2048]
                    k.mm(pT_, p_[:], [(w[:, kc * 128:(kc + 1) * 128], aT[:, 16 + kc, :]) for kc in range(KC)], [sT, mg_T])
                    evac_y(pT_, p_, oc, oc == 0, oc == 15)
            post_norm_residual(GG1)
            make_h(G2, 48)
            for ug in range(22):
                slot, sT = wload(WU_d[l, ug], 8192)
                for pi in range(2):
                    j = 2 * ug + pi
                    accs = []
                    for gv in range(2):
                        ci = 2 * j + gv
                        pT_, p_ = proj_fm(slot, sT, 2 * pi + gv, hT, hT_T, KC, 0)
                        Zb, Zb_T = zb[ci % 2], zb_T[ci % 2]
                        k.op('act', lambda e, Zb=Zb, ci=ci: e.activation(out=Zb[:, 0:2], in_=carry[:, ci, :], func=AF.Copy), [carry_T], [Zb_T])
                        k.op('act', lambda e, Zb=Zb, p_=p_: e.activation(out=Zb[:, 2:2 + TT], in_=p_[:], func=AF.Copy), [pT_], [Zb_T])
                        k.op('act', lambda e, Zb=Zb, ci=ci: e.activation(out=carry[:, ci, :], in_=Zb[:, TT:TT + 2], func=AF.Copy), [Zb_T], [carry_T])
                        ac, ac_T = [(tmpA, tmpA_T), (tmpB, tmpB_T)][gv]
                        k.op('act', lambda e, p_=p_, ac=ac, ci=ci: e.activation(out=ac[:], in_=p_[:], func=AF.Identity, scale=cw[:, 3 * ci + 2:3 * ci + 3], bias=cb[:, ci:ci + 1]), [pT_, LP], [ac_T])
                        k.op('dve', lambda e, Zb=Zb, ac=ac, ci=ci: e.scalar_tensor_tensor(out=ac[:], in0=Zb[:, 1:1 + TT], scalar=cw[:, 3 * ci + 1:3 * ci + 2], in1=ac[:], op0=ALU.mult, op1=ALU.add), [Zb_T, LP, ac_T], [ac_T])
                        k.op('dve', lambda e, Zb=Zb, ac=ac, ci=ci: e.scalar_tensor_tensor(out=ac[:], in0=Zb[:, 0:TT], scalar=cw[:, 3 * ci:3 * ci + 1], in1=ac[:], op0=ALU.mult, op1=ALU.add), [Zb_T, LP, ac_T], [ac_T])
                    k.op('act', lambda e: e.activation(out=tmpA[:], in_=tmpA[:], func=GELU), [tmpA_T], [tmpA_T])
                    k.op('dve', lambda e, j=j: e.tensor_tensor(out=aT[:, j, :], in0=tmpA[:], in1=tmpB[:], op=ALU.mult), [tmpA_T, tmpB_T], [aT_T])
            for oc in range(16):
                slot, sT = wload(WD_d[l, oc], FC * 128)
                pT_, p_ = PS()
                k.mm(pT_, p_[:], [(slot[:, kc * 128:(kc + 1) * 128], aT[:, kc, :]) for kc in range(FC)], [sT, aT_T])
                evac_y(pT_, p_, oc, oc == 0, oc == 15)
            post_norm_residual(GG2)
            for c in range(KC):
                k.dma('sp', Xdst[c, :, c0:c0 + TT], xT[:, c, :], [xT_T], [XdstT[ti]])
            for t_ in (gu_T, ys_T, ym_T, yp_T, mg_T, vn_T, p4_T, pl_T):
                t_.r.update(aT_T.r); t_.r.update(aT_T.w)
    k.barrier()
    return nc, es


def _fm(v):
    n = v.shape[-1] // 128
    return np.ascontiguousarray(v.reshape(n, 128).T)


def _tile_w(W):
    K, N = W.shape
    nk, nj = K // 128, N // 128
    return np.ascontiguousarray(W.reshape(nk, 128, nj, 128).transpose(2, 1, 0, 3)).reshape(nj, 128, nk * 128)


def _prep_shared(inp, SEQ, DEPTH):
    f32 = np.float32
    bf = ml_dtypes.bfloat16
    L = DEPTH
    m = {}
    col_order = np.concatenate([np.concatenate([np.arange(j * 128, (j + 1) * 128), DFF + np.arange(j * 128, (j + 1) * 128)]) for j in range(FC)])
    gl = lambda n: np.asarray(inp[n], dtype=f32)
    m["gn"] = np.stack([np.concatenate([_fm(gl(n)[l]) for n in ("g_pre_mix", "g_post_mix", "g_pre_ffn", "g_post_ffn")], axis=1) for l in range(L)])
    m["bada"] = np.stack([_fm(gl("b_ada")[l]) for l in range(L)])
    m["binT"] = np.stack([_fm(gl("b_in")[l]) for l in range(L)])
    bc = lambda v: np.ascontiguousarray(np.broadcast_to(v[None, :], (128, v.shape[0])))
    m["bvB"] = np.stack([bc(gl("b_in")[l][512:1024]) for l in range(L)])
    m["bvaB"] = np.stack([bc(gl("b_in")[l][2048:2560]) for l in range(L)])
    m["sngB"] = np.stack([bc(gl("sgu_norm_g")[l]) for l in range(L)])
    sb_ = gl("sgu_b")
    m["sbT"] = np.ascontiguousarray(np.stack([np.stack([np.concatenate([np.broadcast_to(sb_[l, 2 * cc][None], (64, 128)), np.broadcast_to(sb_[l, 2 * cc + 1][None], (64, 128))], 0) for cc in range(4)], 1) for l in range(L)]))
    m["swT"] = np.ascontiguousarray(gl("sgu_w").transpose(0, 3, 1, 2))
    m["pw"] = np.ascontiguousarray(gl("pool_w").transpose(0, 2, 1, 3))
    m["psc"] = np.stack([_fm(gl("pool_scale")[l]) for l in range(L)])
    wc = gl("w_conv")[:, :, col_order]
    m["cw"] = np.ascontiguousarray(wc.reshape(L, 3, 88, 128).transpose(0, 3, 2, 1)).reshape(L, 128, 264)
    m["cb"] = np.stack([_fm(gl("b_conv")[l][col_order]) for l in range(L)])
    m["WA"] = np.stack([_tile_w(gl("w_ada")[l]).reshape(48, 2, 128, 2048).transpose(0, 2, 1, 3).reshape(48, 128, 4096) for l in range(L)])
    WI, WM, WO, WU, WD = [], [], [], [], []
    for l in range(L):
        tw = _tile_w(gl("w_in")[l])
        order = [0, 8, 12, 16, 4, 20]
        WI.append(np.stack([tw[o:o + 4].transpose(1, 0, 2).reshape(128, 8192) for o in order]))
        brs = [_tile_w(gl(n)[l]) for n in ("w_sgu_out", "w_moba_out", "w_pool_out")]
        WM.append(np.stack([np.concatenate([tw[24 + br * 16 + oc] for br in range(3)] + [brs[br][oc] for br in range(3)], axis=1) for oc in range(16)]))
        two = _tile_w(gl("w_out")[l])
        WO.append(np.stack([two[4 * g:4 * g + 4].transpose(1, 0, 2).reshape(128, 8192) for g in range(4)]))
        twu = _tile_w(gl("w_up")[l][:, col_order])
        WU.append(np.stack([twu[4 * g:4 * g + 4].transpose(1, 0, 2).reshape(128, 8192) for g in range(22)]))
        WD.append(_tile_w(gl("w_down")[l]))
    m["WI"], m["WM"], m["WO"], m["WU"], m["WD"] = (np.ascontiguousarray(np.stack(a)) for a in (WI, WM, WO, WU, WD))
    pos = np.arange(SEQ)
    kaux = np.zeros((8, 36, SEQ), f32); qaux = np.zeros((8, 4, SEQ), f32)
    for h in range(8):
        f = 2.0 ** (2 - h)
        for n in range(NBP):
            kaux[h, n] = (pos // 256 == n)
        kaux[h, 32] = f * (pos // 32 * 32); kaux[h, 33] = f * (pos % 32); kaux[h, 34:36] = 1.0
        qaux[h, 0:2] = 1.0; qaux[h, 2] = -f * (pos // 32 * 32); qaux[h, 3] = -f * (pos % 32)
    m["kaux"] = kaux.astype(bf); m["qaux"] = qaux.astype(bf)
    n_ = np.arange(NBP)
    m["blkb"] = np.ascontiguousarray(np.broadcast_to(np.where(n_[None, :] < n_[:, None], 0.0, -1e30).astype(f32)[None], (128, NBP, NBP)))
    m["owni"] = np.ascontiguousarray(np.broadcast_to((n_[None, :] == n_[:, None]).astype(f32)[None], (128, NBP, NBP)))
    p = np.arange(128)
    xx = np.arange(512)
    m["cm"] = np.stack([(xx[None, :] >= 128 * j + p[:, None]) for j in range(4)], 1).astype(f32).astype(bf)
    m["onesb"] = np.ones((128, 128), f32).astype(bf)
    m["ident"] = np.eye(128, dtype=f32).astype(bf)
    sh = np.zeros((128, 64), f32); sh[64 + np.arange(64), np.arange(64)] = 1.0
    m["shsel"] = sh.astype(bf)
    m["invc"] = np.ascontiguousarray(np.broadcast_to(np.stack([1.0 / np.minimum(xx + 1, w) for w in (2, 4, 8, 16)], 0).astype(f32)[None], (128, 4, 512)))
    m["triu"] = (p[:, None] <= p[None, :]).astype(f32)
    return m


def run(inputs, SEQ, DEPTH, n_cores=8):
    nc, es = build(SEQ, DEPTH)
    shared = _prep_shared(inputs, SEQ, DEPTH)
    x = np.asarray(inputs["x"], dtype=np.float32)
    c = np.asarray(inputs["c"], dtype=np.float32)
    B = x.shape[0]
    in_maps = []
    for core in range(n_cores):
        b = core % B
        mm_ = dict(shared)
        mm_["xT"] = np.ascontiguousarray(x[b].T).reshape(KC, 128, SEQ)
        mm_["cT"] = np.ascontiguousarray(c[b].reshape(KC, 128).T)
        in_maps.append(mm_)
    res = run_bass_kernel_spmd(nc, in_maps, core_ids=list(range(n_cores)))
    out = np.stack([np.ascontiguousarray(res.results[b]["outT"].reshape(D, SEQ).T) for b in range(B)])
    es.close()
    return out.astype(np.float32)


def kernel(**inputs):
    return run(inputs, 8192, 2, 8)
```
